# Optimizing a Trainium2 kernel written in Bass

```python
import jax
import jax.numpy as jnp
from jax import lax
import numpy as np

D_MODEL = 1024
BATCH = 32
SEQ = 2048
DEPTH = 1

HEAD_DIM = 64
N_HEADS_DIL = 8
DIL_PATTERNS = ((128, 1), (512, 4), (2048, 16))
DIL_BLOCK = 128
N_HEADS_NSA = 8
N_KV_NSA = 2
CMP_STRIDE = 16
CMP_LEN = 2 * CMP_STRIDE
CMP_HIDDEN = 256
SEL_LEN = 64
N_SEL = 8
SEL_QCHUNK = 64
WIN = 512
WIN_BLOCK = 128
ROPE_THETA = 500000.0
ROT_DIM = HEAD_DIM // 4
N_GROUPS = 8
EXPERTS_PER_GROUP = 8
N_EXPERTS = N_GROUPS * EXPERTS_PER_GROUP
TOP_K_INNER = 2
D_FF_EXPERT = 256
EPS = 1e-6
NEG = -1e30
BIG = 1e9
SPLIT_SIZES = (N_HEADS_DIL * HEAD_DIM, N_HEADS_DIL * HEAD_DIM, N_HEADS_DIL * HEAD_DIM,
               N_HEADS_NSA * HEAD_DIM,
               N_KV_NSA * HEAD_DIM, N_KV_NSA * HEAD_DIM, N_KV_NSA * HEAD_DIM,
               N_KV_NSA * HEAD_DIM, N_KV_NSA * HEAD_DIM, N_KV_NSA * HEAD_DIM,
               N_HEADS_NSA * 3)
D_IN = sum(SPLIT_SIZES)
D_MIX = (N_HEADS_DIL + N_HEADS_NSA) * HEAD_DIM

kernel_name = 'hymba_dilated_nsa_hmoe_block'


def rmsnorm(x, g):
    xf = x.astype(jnp.float32)
    y = xf * lax.rsqrt(jnp.mean(xf * xf, axis=-1, keepdims=True) + EPS) * g.astype(jnp.float32)
    return y.astype(x.dtype)


def rope_tables(pos):
    inv_freq = ROPE_THETA ** (-jnp.arange(0, ROT_DIM, 2, dtype=jnp.float32) / ROT_DIM)
    ang = pos.astype(jnp.float32)[:, None] * inv_freq[None, :]
    return jnp.cos(ang), jnp.sin(ang)


def apply_rope(x, cos, sin):
    half = ROT_DIM // 2
    xf = x.astype(jnp.float32)
    x1, x2 = xf[..., :half], xf[..., half:ROT_DIM]
    out = jnp.concatenate([x1 * cos - x2 * sin, x2 * cos + x1 * sin, xf[..., ROT_DIM:]], axis=-1)
    return out.astype(x.dtype)


def banded_attention(q, k, v, max_dist, block):
    B_, G, R, L, hd = q.shape
    n_prev = -(-max_dist // block)
    nb = -(-L // block)
    Lp = nb * block
    span = (n_prev + 1) * block
    qp = jnp.pad(q, ((0, 0), (0, 0), (0, 0), (0, Lp - L), (0, 0)))
    kp = jnp.pad(k, ((0, 0), (0, 0), (n_prev * block, Lp - L), (0, 0)))
    vp = jnp.pad(v, ((0, 0), (0, 0), (n_prev * block, Lp - L), (0, 0)))
    scale = hd ** -0.5

    def one_block(i):
        start = i * block
        qb = lax.dynamic_slice_in_dim(qp, start, block, axis=3)
        kb = lax.dynamic_slice_in_dim(kp, start, span, axis=2)
        vb = lax.dynamic_slice_in_dim(vp, start, span, axis=2)
        qpos = start + jnp.arange(block)
        kpos = start - n_prev * block + jnp.arange(span)
        dist = qpos[:, None] - kpos[None, :]
        mask = (dist >= 0) & (dist <= max_dist) & (kpos[None, :] >= 0)
        s = jnp.einsum('bgrqd,bgkd->bgrqk', qb, kb).astype(jnp.float32) * scale
        s = jnp.where(mask, s, NEG)
        m = jnp.max(s, axis=-1, keepdims=True)
        p = jnp.exp(s - m)
        l = jnp.sum(p, axis=-1, keepdims=True)
        o = jnp.einsum('bgrqk,bgkd->bgrqd', p.astype(vb.dtype), vb).astype(jnp.float32) / l
        return o.astype(q.dtype), (m + jnp.log(l))[..., 0]

    o, lse = lax.map(one_block, jnp.arange(nb))
    o = jnp.moveaxis(o, 0, 3).reshape(B_, G, R, Lp, hd)[:, :, :, :L]
    lse = jnp.moveaxis(lse, 0, 3).reshape(B_, G, R, Lp)[..., :L]
    return o, lse


def dilated_mixer(q, k, v):
    B_, H, S, hd = q.shape
    outs, lses = [], []
    for window, dil in DIL_PATTERNS:
        L = S // dil

        def to_sub(t, L=L, dil=dil):
            return t.reshape(B_, H, L, dil, hd).transpose(0, 1, 3, 2, 4).reshape(B_, H * dil, L, hd)

        o, lse = banded_attention(to_sub(q)[:, :, None], to_sub(k), to_sub(v), window // dil, DIL_BLOCK)
        outs.append(o[:, :, 0].reshape(B_, H, dil, L, hd).transpose(0, 1, 3, 2, 4).reshape(B_, H, S, hd))
        lses.append(lse[:, :, 0].reshape(B_, H, dil, L).transpose(0, 1, 3, 2).reshape(B_, H, S))
    w = jax.nn.softmax(jnp.stack(lses, axis=0), axis=0)
    out = jnp.sum(w[..., None] * jnp.stack(outs, axis=0).astype(jnp.float32), axis=0)
    return out.astype(q.dtype)


def compress_kv(kv, pe, w1, w2):
    B_, S, G, hd = kv.shape
    chunks = kv.reshape(B_, S // CMP_STRIDE, CMP_STRIDE, G, hd)
    blocks = jnp.concatenate([chunks[:, :-1], chunks[:, 1:]], axis=2) + pe[None, None, :, None, :]
    n_c = blocks.shape[1]
    flat = blocks.transpose(0, 1, 3, 2, 4).reshape(B_, n_c, G, CMP_LEN * hd)
    out = jax.nn.gelu(flat @ w1) @ w2
    return out.transpose(0, 2, 1, 3)


def nsa_mixer(q, kc_raw, vc_raw, ks, vs, kw, vw, gate_logits, pe_kc, w_kc1, w_kc2, pe_vc, w_vc1, w_vc2):
    B_, G, R, S, hd = q.shape
    scale = hd ** -0.5
    t = jnp.arange(S)

    n_c = S // CMP_STRIDE - 1
    end_pos = jnp.arange(n_c) * CMP_STRIDE + CMP_LEN - 1
    cos_c, sin_c = rope_tables(end_pos)
    kc = apply_rope(compress_kv(kc_raw, pe_kc, w_kc1, w_kc2), cos_c, sin_c)
    vc = compress_kv(vc_raw, pe_vc, w_vc1, w_vc2)
    cmask = end_pos[None, :] <= t[:, None]
    s = jnp.einsum('bgrsd,bgcd->bgrsc', q, kc).astype(jnp.float32) * scale
    s = jnp.where(cmask, s, NEG)
    m = jnp.max(s, axis=-1, keepdims=True)
    p = jnp.where(cmask, jnp.exp(s - m), 0.0)
    l = jnp.sum(p, axis=-1, keepdims=True)
    p_cmp = p / jnp.where(l > 0, l, 1.0)
    o_cmp = jnp.einsum('bgrsc,bgcd->bgrsd', p_cmp.astype(vc.dtype), vc)

    n_s = S // SEL_LEN
    cs = jnp.arange(n_c) * CMP_STRIDE
    js = jnp.arange(n_s) * SEL_LEN
    overlap = jnp.clip(jnp.minimum(cs[:, None] + CMP_LEN, js[None, :] + SEL_LEN)
                       - jnp.maximum(cs[:, None], js[None, :]), 0, None).astype(jnp.float32) / CMP_LEN
    imp = jnp.einsum('bgrsc,cj->bgsj', p_cmp, overlap)
    j = jnp.arange(n_s)[None, :]
    blk_t = (t // SEL_LEN)[:, None]
    forced = (j == 0) | (j == blk_t) | (j == blk_t - 1)
    valid = j * SEL_LEN <= t[:, None]
    imp = jnp.where(valid, jnp.where(forced, BIG, imp), -BIG)
    k_sel = min(N_SEL, n_s)
    _, idx = lax.top_k(imp, k_sel)

    ks_blocks = ks.reshape(B_, G, n_s, SEL_LEN, hd)
    vs_blocks = vs.reshape(B_, G, n_s, SEL_LEN, hd)
    gather = jax.vmap(jax.vmap(lambda blk, ids: blk[ids]))

    def sel_chunk(c):
        q0 = c * SEL_QCHUNK
        qc = lax.dynamic_slice_in_dim(q, q0, SEL_QCHUNK, axis=3)
        ic = lax.dynamic_slice_in_dim(idx, q0, SEL_QCHUNK, axis=2)
        kb = gather(ks_blocks, ic)
        vb = gather(vs_blocks, ic)
        kpos = ic[..., None] * SEL_LEN + jnp.arange(SEL_LEN)
        qpos = q0 + jnp.arange(SEL_QCHUNK)
        msk = kpos <= qpos[None, None, :, None, None]
        sc = jnp.einsum('bgrqd,bgqkjd->bgrqkj', qc, kb).astype(jnp.float32) * scale
        sc = jnp.where(msk[:, :, None], sc, NEG).reshape(B_, G, R, SEL_QCHUNK, k_sel * SEL_LEN)
        pc = jax.nn.softmax(sc, axis=-1)
        return jnp.einsum('bgrqn,bgqnd->bgrqd', pc.astype(vb.dtype),
                          vb.reshape(B_, G, SEL_QCHUNK, k_sel * SEL_LEN, hd))

    o_sel = lax.map(sel_chunk, jnp.arange(S // SEL_QCHUNK))
    o_sel = jnp.moveaxis(o_sel, 0, 3).reshape(B_, G, R, S, hd)

    o_win, _ = banded_attention(q, kw, vw, WIN - 1, WIN_BLOCK)

    g = jax.nn.sigmoid(gate_logits.astype(jnp.float32)).reshape(B_, S, G, R, 3).transpose(0, 2, 3, 1, 4)
    out = (g[..., 0:1] * o_cmp.astype(jnp.float32) + g[..., 1:2] * o_sel.astype(jnp.float32)
           + g[..., 2:3] * o_win.astype(jnp.float32))
    return out.astype(q.dtype)


def hier_moe(h, w_rg, b_rg, w_re, b_re, w_gate, w_up, w_down):
    T = h.shape[0]
    pg = jax.nn.softmax((h @ w_rg + b_rg).astype(jnp.float32), axis=-1)
    g_star = jnp.argmax(pg, axis=-1)
    p_g = jnp.max(pg, axis=-1, keepdims=True)
    g_onehot = jax.nn.one_hot(g_star, N_GROUPS, dtype=jnp.float32)
    el = (jnp.einsum('td,dge->tge', h, w_re) + b_re).astype(jnp.float32)
    el_sel = jnp.einsum('tge,tg->te', el, g_onehot)
    pe = jax.nn.softmax(el_sel, axis=-1)
    top_v, top_i = lax.top_k(pe, TOP_K_INNER)
    top_w = top_v / jnp.sum(top_v, axis=-1, keepdims=True) * p_g
    inner = jnp.sum(jax.nn.one_hot(top_i, EXPERTS_PER_GROUP, dtype=jnp.float32) * top_w[..., None], axis=1)
    comb = (g_onehot[:, :, None] * inner[:, None, :]).reshape(T, N_EXPERTS)
    y = jnp.zeros((T, h.shape[1]), jnp.float32)
    for e in range(N_EXPERTS):
        he = jax.nn.silu(h @ w_gate[e]) * (h @ w_up[e])
        y = y + comb[:, e:e + 1] * (he @ w_down[e]).astype(jnp.float32)
    return y.astype(h.dtype)


def hybrid_layer(x, cos_t, sin_t, norm1_g, w_in, pe_kc, w_kc1, w_kc2, pe_vc, w_vc1, w_vc2, w_o,
                 norm2_g, w_rg, b_rg, w_re, b_re, w_gate, w_up, w_down):
    B_, S, D = x.shape
    hd = HEAD_DIM
    h = rmsnorm(x, norm1_g)
    proj = h @ w_in
    offsets = [int(o) for o in np.cumsum(SPLIT_SIZES)[:-1]]
    aq, ak, av, bq, kc_raw, vc_raw, ks, vs, kw, vw, gate_logits = jnp.split(proj, offsets, axis=-1)

    def heads(t, n):
        return t.reshape(B_, S, n, hd).transpose(0, 2, 1, 3)

    o_a = dilated_mixer(apply_rope(heads(aq, N_HEADS_DIL), cos_t, sin_t),
                        apply_rope(heads(ak, N_HEADS_DIL), cos_t, sin_t),
                        heads(av, N_HEADS_DIL))
    R = N_HEADS_NSA // N_KV_NSA
    qb = apply_rope(bq.reshape(B_, S, N_KV_NSA, R, hd).transpose(0, 2, 3, 1, 4), cos_t, sin_t)
    o_b = nsa_mixer(qb, kc_raw.reshape(B_, S, N_KV_NSA, hd), vc_raw.reshape(B_, S, N_KV_NSA, hd),
                    apply_rope(heads(ks, N_KV_NSA), cos_t, sin_t), heads(vs, N_KV_NSA),
                    apply_rope(heads(kw, N_KV_NSA), cos_t, sin_t), heads(vw, N_KV_NSA),
                    gate_logits, pe_kc, w_kc1, w_kc2, pe_vc, w_vc1, w_vc2)
    mix = jnp.concatenate([o_a.transpose(0, 2, 1, 3).reshape(B_, S, -1),
                           o_b.transpose(0, 3, 1, 2, 4).reshape(B_, S, -1)], axis=-1)
    x = x + mix @ w_o
    h2 = rmsnorm(x, norm2_g).reshape(B_ * S, D)
    return x + hier_moe(h2, w_rg, b_rg, w_re, b_re, w_gate, w_up, w_down).reshape(B_, S, D)


def setup_inputs(seed: int = 0) -> dict:
    key = jax.random.key(seed)
    ks = jax.random.split(key, 20)
    f32 = jnp.float32

    def nrm(k, shape, fan_in):
        return jax.random.normal(k, shape, f32) * fan_in ** -0.5

    return {
        'x': jax.random.normal(ks[0], (BATCH, SEQ, D_MODEL), f32),
        'norm1_g': 1.0 + 0.02 * jax.random.normal(ks[1], (DEPTH, D_MODEL), f32),
        'w_in': nrm(ks[2], (DEPTH, D_MODEL, D_IN), D_MODEL),
        'pe_kc': 0.1 * jax.random.normal(ks[3], (DEPTH, CMP_LEN, HEAD_DIM), f32),
        'w_kc1': nrm(ks[4], (DEPTH, CMP_LEN * HEAD_DIM, CMP_HIDDEN), CMP_LEN * HEAD_DIM),
        'w_kc2': nrm(ks[5], (DEPTH, CMP_HIDDEN, HEAD_DIM), CMP_HIDDEN),
        'pe_vc': 0.1 * jax.random.normal(ks[6], (DEPTH, CMP_LEN, HEAD_DIM), f32),
        'w_vc1': nrm(ks[7], (DEPTH, CMP_LEN * HEAD_DIM, CMP_HIDDEN), CMP_LEN * HEAD_DIM),
        'w_vc2': nrm(ks[8], (DEPTH, CMP_HIDDEN, HEAD_DIM), CMP_HIDDEN),
        'w_o': nrm(ks[9], (DEPTH, D_MIX, D_MODEL), D_MIX),
        'norm2_g': 1.0 + 0.02 * jax.random.normal(ks[10], (DEPTH, D_MODEL), f32),
        'w_rg': nrm(ks[11], (DEPTH, D_MODEL, N_GROUPS), D_MODEL),
        'b_rg': 0.01 * jax.random.normal(ks[12], (DEPTH, N_GROUPS), f32),
        'w_re': nrm(ks[13], (DEPTH, D_MODEL, N_GROUPS, EXPERTS_PER_GROUP), D_MODEL),
        'b_re': 0.01 * jax.random.normal(ks[14], (DEPTH, N_GROUPS, EXPERTS_PER_GROUP), f32),
        'w_gate': nrm(ks[15], (DEPTH, N_EXPERTS, D_MODEL, D_FF_EXPERT), D_MODEL),
        'w_up': nrm(ks[16], (DEPTH, N_EXPERTS, D_MODEL, D_FF_EXPERT), D_MODEL),
        'w_down': nrm(ks[17], (DEPTH, N_EXPERTS, D_FF_EXPERT, D_MODEL), D_FF_EXPERT),
        'norm_f_g': 1.0 + 0.02 * jax.random.normal(ks[18], (D_MODEL,), f32),
    }


def reference(x, norm1_g, w_in, pe_kc, w_kc1, w_kc2, pe_vc, w_vc1, w_vc2, w_o, norm2_g,
              w_rg, b_rg, w_re, b_re, w_gate, w_up, w_down, norm_f_g):
    S = x.shape[1]
    cos_t, sin_t = rope_tables(jnp.arange(S))
    for l in range(DEPTH):
        x = hybrid_layer(x, cos_t, sin_t, norm1_g[l], w_in[l], pe_kc[l], w_kc1[l], w_kc2[l],
                         pe_vc[l], w_vc1[l], w_vc2[l], w_o[l], norm2_g[l], w_rg[l], b_rg[l],
                         w_re[l], b_re[l], w_gate[l], w_up[l], w_down[l])
    return rmsnorm(x, norm_f_g)
```

```python
import contextlib
import numpy as np
import ml_dtypes
import concourse.bass as bass
import concourse.mybir as mybir
from concourse.ap import AP
from concourse.bass_utils import run_bass_kernel_spmd

F32 = mybir.dt.float32
BF16 = mybir.dt.bfloat16
U8 = mybir.dt.uint8
I32 = mybir.dt.int32
from concourse.bass import IndirectOffsetOnAxis
ALU = mybir.AluOpType
AF = mybir.ActivationFunctionType
AX = mybir.AxisListType

D = 1024
S_LEN = 2048
NT = 16
HD = 64
N_CORES = 8
B_TOTAL = 32
D_IN = 2840
NKV = 1792
NQ = 1048
NE = 64
DFF = 256
EPS = 1e-6
NEGM = -30000.0
import os
CUT = int(os.environ.get("KCUT", "9"))
STRICT = os.environ.get("KSTRICT", "1") == "1"

CB_ID = 0
CB_MALL = 128
CB_CM = CB_MALL + 2048
CB_TRI = CB_CM + 2048
CB_TRIW = CB_TRI + 128
CB_E = CB_TRIW + 128
CB_UT = CB_E + 2048
CB_ONES = CB_UT + 128
CB_N = CB_ONES + 128
CF_SA = 0
CF_SB = 512
CF_COS = 1024
CF_SIN = 1152
CF_COSC = 1280
CF_SINC = 1288
CF_OV = 1296
CF_THR = 1328
CF_SIDX = 1360
CF_PIDX = 1488
CF_N = 1492
NSLOT = 128
SROWS = 256
NHALF = NSLOT * 2


class Sync:
    NDMA = 40

    def __init__(self, nc, es):
        self.nc = nc
        self.engs = {"pe": nc.tensor, "dve": nc.vector, "act": nc.scalar, "pool": nc.gpsimd, "sp": nc.sync}
        self.sem = {k: es.enter_context(nc.semaphore(f"sem_{k}")) for k in ("pe", "dve", "act", "pool")}
        self.cnt = {k: 0 for k in self.sem}
        self.pending = {k: False for k in self.sem}
        self.waited = {k: {} for k in self.engs}
        self.dsem = [es.enter_context(nc.semaphore(f"dsem{i}")) for i in range(self.NDMA)]
        self.dcnt = [0] * self.NDMA
        self.dnext = {"sp": 0, "pool": 0, "act": 0}
        self.drange = {"sp": (0, 24), "pool": (24, 40), "act": (0, 24)}
        self.lastw = {}
        self.readers = {}
        self.all_dma_tokens = {}
        self.n_ins = 0
        self.n_wait = 0

    def _semobj(self, k):
        return self.sem[k] if isinstance(k, str) else self.dsem[k]

    def _wait(self, eng, tok, raw):
        if tok is None:
            return
        k, v = tok
        if k == eng:
            if eng == "pe" or (not raw and not STRICT):
                return
        if self.waited[eng].get(k, 0) >= v:
            return
        self.engs[eng].wait_ge(self._semobj(k), v)
        self.waited[eng][k] = v
        self.n_wait += 1

    def _deps(self, eng, reads, writes):
        for b in reads:
            self._wait(eng, self.lastw.get(b), True)
            if isinstance(b, tuple) and b[0] == "ps":
                for tk in self.readers.get(b, ()):
                    self._wait(eng, tk, False)
        for b in writes:
            self._wait(eng, self.lastw.get(b), False)
            for tk in self.readers.get(b, ()):
                self._wait(eng, tk, False)

    def _record(self, tok, reads, writes):
        for b in reads:
            lst = self.readers.setdefault(b, [])
            lst[:] = [t for t in lst if t[0] != tok[0]]
            lst.append(tok)
        for b in writes:
            self.lastw[b] = tok
            self.readers[b] = []

    def op(self, eng, fn, reads=(), writes=(), inc=True):
        self._deps(eng, reads, writes)
        ins = fn(self.engs[eng])
        self.n_ins += 1
        if inc:
            self.cnt[eng] += 1
            ins.then_inc(self.sem[eng], 1)
            tok = (eng, self.cnt[eng])
            self.pending[eng] = False
        else:
            tok = (eng, self.cnt[eng] + 1)
            self.pending[eng] = True
        self._record(tok, reads, writes)
        return tok

    def dma(self, q, out, in_, reads=(), writes=(), **kw):
        lo_, hi_ = self.drange[q]
        j = lo_ + self.dnext[q]
        self.dnext[q] = (self.dnext[q] + 1) % (hi_ - lo_)
        if self.dcnt[j] > 0:
            self._wait(q, (j, 16 * self.dcnt[j]), False)
        self._deps(q, reads, writes)
        if q == "pool":
            kw.setdefault("max_dma_last_dim", 2048)
        ins = self.engs[q].dma_start(out=out, in_=in_, **kw)
        self.dcnt[j] += 1
        ins.then_inc(self.dsem[j], 16)
        tok = (j, 16 * self.dcnt[j])
        self.all_dma_tokens[j] = tok
        self.n_ins += 1
        self._record(tok, reads, writes)
        return tok

    def idma(self, out, out_off, in_, in_off, reads=(), writes=(), **kw):
        q = "pool"
        lo_, hi_ = self.drange[q]
        j = lo_ + self.dnext[q]
        self.dnext[q] = (self.dnext[q] + 1) % (hi_ - lo_)
        if self.dcnt[j] > 0:
            self._wait(q, (j, 16 * self.dcnt[j]), False)
        self._deps(q, reads, writes)
        ins = self.engs[q].indirect_dma_start(out=out, out_offset=out_off, in_=in_, in_offset=in_off, **kw)
        self.dcnt[j] += 1
        ins.then_inc(self.dsem[j], 16)
        tok = (j, 16 * self.dcnt[j])
        self.all_dma_tokens[j] = tok
        self.n_ins += 1
        self._record(tok, reads, writes)
        return tok

    def barrier(self):
        for k in self.sem:
            assert not self.pending[k], f"pending non-inc instruction on {k}"
        for e in self.engs:
            for k in self.sem:
                if self.cnt[k] > 0:
                    self._wait(e, (k, self.cnt[k]), True)
            for j, tok in self.all_dma_tokens.items():
                self._wait(e, tok, True)
        self.lastw.clear()
        self.readers.clear()


def mk_ap(base, dims):
    return AP(base.tensor, base.offset, [list(base.ap[0])] + [list(d) for d in dims])


def host_constants():
    cb = np.zeros((128, CB_N), np.float32)
    cb[:, CB_ID:CB_ID + 128] = np.eye(128)
    ki = np.arange(128)[:, None]
    qi = np.arange(128)[None, :]
    for m in range(16):
        dist = (15 - m) * 128 + qi - ki
        c = ((dist >= 0) & (dist <= 128)).astype(np.float32)
        c += ((dist >= 0) & (dist % 4 == 0) & (dist <= 512))
        c += ((dist >= 0) & (dist % 16 == 0) & (dist <= 2048))
        cb[:, CB_MALL + m * 128:CB_MALL + (m + 1) * 128] = c
    cidx = np.arange(128)[:, None]
    for t in range(16):
        q = t * 128 + qi
        cb[:, CB_CM + t * 128:CB_CM + (t + 1) * 128] = ((16 * cidx + 31 <= q) & (cidx < 127))
    cb[:, CB_TRI:CB_TRI + 128] = (ki <= qi)
    cb[:, CB_TRIW:CB_TRIW + 128] = (qi < ki)
    kk = np.arange(2048)[None, :]
    jj = np.arange(128)[:, None]
    cb[:, CB_E:CB_E + 2048] = ((kk // 64 == (jj % 64)) & ((jj % 64) < 32))
    cb[:, CB_UT:CB_UT + 128] = (ki < qi)
    cb[:, CB_ONES:CB_ONES + 128] = 1.0
    cf = np.zeros((128, CF_N), np.float32)
    cf[:, CF_THR:CF_THR + 32] = float(SROWS) * np.arange(32)[None, :]
    cf[:, CF_SIDX:CF_SIDX + NSLOT] = np.arange(NSLOT)[None, :]
    cf[:, CF_PIDX] = np.arange(128)
    p = np.arange(128)[:, None]
    j = np.arange(32)[None, :]
    for t in range(16):
        tq = t * 128 + p
        blk = tq // 64
        forced = (j == 0) | (j == blk) | (j == blk - 1)
        valid = (j * 64 <= tq)
        cf[:, CF_SA + t * 32:CF_SA + (t + 1) * 32] = (valid & ~forced)
        cf[:, CF_SB + t * 32:CF_SB + (t + 1) * 32] = np.where(valid, np.where(forced, 1e9, 0.0), -1e9)
    inv_freq = (500000.0 ** (-np.arange(0, 16, 2, dtype=np.float32) / 16)).astype(np.float32)
    pos = np.arange(2048, dtype=np.float32)
    ang = pos[:, None] * inv_freq[None, :]
    cos = np.cos(ang).astype(np.float32).reshape(16, 128, 8).transpose(1, 0, 2).reshape(128, 128)
    sin = np.sin(ang).astype(np.float32).reshape(16, 128, 8).transpose(1, 0, 2).reshape(128, 128)
    cf[:, CF_COS:CF_COS + 128] = cos
    cf[:, CF_SIN:CF_SIN + 128] = sin
    endp = (np.arange(127) * 16 + 31).astype(np.float32)
    angc = endp[:, None] * inv_freq[None, :]
    cf[:127, CF_COSC:CF_COSC + 8] = np.cos(angc)
    cf[:127, CF_SINC:CF_SINC + 8] = np.sin(angc)
    cs = np.arange(127)[:, None] * 16
    js = np.arange(32)[None, :] * 64
    ov = np.clip(np.minimum(cs + 32, js + 64) - np.maximum(cs, js), 0, None).astype(np.float32) / 32
    cf[:127, CF_OV:CF_OV + 32] = ov
    return cb.astype(ml_dtypes.bfloat16), cf


def w_in_perm():
    aq, ak, av, bq, kc, vc, ks, vs, kw, vw, gt = 0, 512, 1024, 1536, 2048, 2176, 2304, 2432, 2560, 2688, 2816
    r = lambda a, n: list(range(a, a + n))
    kv = r(ak, 512) + r(ks, 128) + r(kw, 128) + r(kc, 128) + r(vc, 128) + r(av, 512) + r(vs, 128) + r(vw, 128)
    q = r(aq, 512)
    for rr in range(4):
        for g in range(2):
            q += r(bq + (g * 4 + rr) * 64, 64)
    q += r(gt, 24)
    assert len(kv) == NKV and len(q) == NQ
    return np.array(kv), np.array(q)


def build(nseq, debug=False, stop_after=None):
    nc = bass.Bass("TRN2", target_bir_lowering=False)
    es = contextlib.ExitStack()
    dt = lambda name, shape, dty=F32, kind="ExternalInput": nc.dram_tensor(name, shape, dty, kind=kind).ap()
    x_d = dt("x", [nseq, S_LEN, D])
    out_d = dt("out", [nseq, S_LEN, D], kind="ExternalOutput")
    wkv_d = dt("w_kv", [128, 8, NKV])
    wq_d = dt("w_q", [128, 8, NQ])
    wo_d = dt("w_o", [128, 8, D])
    wr_d = dt("w_r", [128, 8, 72])
    br_d = dt("b_r", [72])
    g1_d = dt("g1", [D])
    g2_d = dt("g2", [D])
    gf_d = dt("gf", [D])
    peT_d = dt("peT", [128, 2, 32])
    w1_d = dt("w_c1", [2, 128, 32, 256])
    w2_d = dt("w_c2", [2, 128, 2, 64])
    NEd = NE if stop_after is None else 1
    wg_d = dt("w_gate", [NEd * 128, 8 * DFF])
    wu_d = dt("w_up", [NEd * 128, 8 * DFF])
    wd_d = dt("w_down", [NEd * 128, 2 * D])
    cb_d = dt("cb", [128, CB_N], BF16)
    cf_d = dt("cf", [128, CF_N])
    xmid_d = nc.dram_tensor("xmid", [nseq * S_LEN, D], F32, kind="Internal").ap()
    h2s_d = nc.dram_tensor("h2s", [nseq * S_LEN, D], BF16, kind="Internal").ap()
    oh_d = nc.dram_tensor("ohd", [nseq, 128, 2 * NT * NE], BF16, kind="Internal").ap()
    hs_d = nc.dram_tensor("hs", [NSLOT * SROWS, D], BF16, kind="Internal").ap()
    ys_d = nc.dram_tensor("ys", [NSLOT * SROWS, D], F32, kind="Internal").ap()
    dbg = {}

    def dbg_out(name, shape, dty=F32):
        dbg[name] = dt("dbg_" + name, shape, dty, kind="ExternalOutput")
        return dbg[name]

    sb = lambda name, shape, dty: es.enter_context(nc.sbuf_tensor("s_" + name, shape, dty))
    cb = sb("cb", [128, CB_N], BF16)
    cf = sb("cf", [128, CF_N], F32)
    g2b = sb("gb", [128, D], F32)
    g1b = g2b
    brb = sb("brb", [128, 72], F32)
    wr = sb("wr", [128, 8, 72], BF16)
    peT = sb("peT", [128, 2, 32], F32)
    w2 = sb("w2", [128, 2, 2, 64], BF16)
    xT = sb("xT", [128, 8, S_LEN], BF16)
    OH = sb("OH", [128, 2, NT, NE], BF16)
    RK = sb("RK", [128, 4, 32], F32)
    WV = sb("WV", [128, 4, 32], F32)
    run = sb("run", [128, NE], F32)
    DESTI = sb("DESTI", [128, 4, 32], I32)
    WIDX = sb("WIDX", [128, NSLOT], I32)
    cum = [sb(f"cum{i}", [128, NE], F32) for i in range(2)]
    ntl = sb("ntl", [128, NE], F32)
    EF = sb("EF", [128, NSLOT], F32)
    KCT = sb("KCT", [128, 128], BF16)
    VCX = sb("VCX", [128, 2, 97], BF16)
    R1_BYTES = 32768 + 16640 + 8320 + NQ * 16
    R1 = sb("R1", [128, R1_BYTES], U8)
    o = 0

    def carve(nbytes):
        nonlocal o
        v = R1[:, o:o + nbytes]
        o += nbytes
        return v

    KT = carve(32768).bitcast(BF16).rearrange("p (b n) -> p b n", b=8)
    Vd = carve(16640).bitcast(BF16).rearrange("p (i h e) -> p i h e", i=NT, h=8)
    VS = carve(8320).bitcast(BF16).rearrange("p (i h e) -> p i h e", i=NT, h=4)
    wq = carve(NQ * 16).bitcast(BF16).rearrange("p (c n) -> p c n", c=8)
    R2_BYTES = NKV * 16
    R2 = sb("R2", [128, R2_BYTES], U8)
    wkv = R2[:, 0:NKV * 16].bitcast(BF16).rearrange("p (c n) -> p c n", c=8)
    w1 = R2[:, 0:16384].bitcast(BF16).rearrange("p (q h) -> p q h", q=32)
    Z = R2[:, 16384:16384 + 8192].bitcast(BF16).rearrange("p (q c) -> p q c", q=32)
    wo = R2[:, 0:16384].bitcast(BF16).rearrange("p (c n) -> p c n", c=8)
    d_f32 = [R1[:, i * 4096:(i + 1) * 4096].bitcast(F32) for i in range(10)]
    d_bf = [R1[:, 40960 + i * 2048:40960 + (i + 1) * 2048].bitcast(BF16) for i in range(6)]
    o2 = 0
    wgs, wus, wds = [], [], []
    for sl in range(2):
        wgs.append(R2[:, o2:o2 + 4096].bitcast(BF16).rearrange("p (c n) -> p c n", c=8)); o2 += 4096
        wus.append(R2[:, o2:o2 + 4096].bitcast(BF16).rearrange("p (c n) -> p c n", c=8)); o2 += 4096
        wds.append(R2[:, o2:o2 + 4096].bitcast(BF16).rearrange("p (c n) -> p c n", c=2)); o2 += 4096
    o3 = 53248
    wgs.append(R1[:, o3:o3 + 4096].bitcast(BF16).rearrange("p (c n) -> p c n", c=8)); o3 += 4096
    wus.append(R1[:, o3:o3 + 4096].bitcast(BF16).rearrange("p (c n) -> p c n", c=8)); o3 += 4096
    wds.append(R1[:, o3:o3 + 4096].bitcast(BF16).rearrange("p (c n) -> p c n", c=2)); o3 += 4096
    assert o <= R1_BYTES and o2 <= R2_BYTES and 40960 + 6 * 2048 <= 53248 and o3 <= R1_BYTES
    xt = [sb(f"xt{i}", [128, D], F32) for i in range(2)]
    xn = [sb(f"xn{i}", [128, D], BF16) for i in range(2)]
    ktm = [sb(f"ktm{i}", [128, 1024], BF16) for i in range(2)]
    st = [sb(f"st{i}", [128, 8], F32) for i in range(2)]
    rt = [sb(f"rt{i}", [128, 4, 128], F32) for i in range(2)]
    QZd = sb("QZd", [128, 8, 128], BF16)
    QZn = sb("QZn", [128, 2, 4, 128], BF16)
    Pb = [sb(f"P{i}", [128, 512], BF16) for i in range(4)]
    mix = xn[1]
    mixT = sb("mixT", [128, 8, 128], BF16)
    gate = sb("gate", [128, 24], F32)
    sm = sb("sm", [128, 64], F32)
    imp = sb("imp", [128, 4, 32], F32)
    selb = sb("selb", [128, 96], BF16)
    xm = sb("xm", [128, D], F32)
    h2 = xn[0]
    rs = sb("rs", [128, 256], F32)
    he = [xn[0][:, 0:512], xn[0][:, 512:1024], xn[1][:, 0:512], xn[1][:, 512:1024]]
    sg = [xm[:, 0:512], xm[:, 512:1024]]
    ot = xt
    ps = es.enter_context(nc.psum_tensor("ps", [128, 8, 512], F32))
    psb = lambda b: ps[:, b, :].bitcast(BF16)

    S = Sync(nc, es)
    ident = cb[:, CB_ID:CB_ID + 128]
    PSK = lambda b: ("ps", b)

    S.dma("sp", cb[:, :], cb_d[:, :], writes=["cb"])
    S.dma("sp", cf[:, :], cf_d[:, :], writes=["cf"])
    S.dma("sp", brb[:, :], br_d.partition_broadcast(128), writes=["brb"])
    S.dma("sp", peT[:, :, :], peT_d[:, :, :], writes=["peT"])
    S.dma("pool", wr[:, :, :], wr_d[:, :, :], writes=["wr"])
    for kv in range(2):
        S.dma("pool", w2[:, kv, :, :], w2_d[kv], writes=["w2"])
    S.barrier()

    eps_t = sb("eps_t", [128, 1], F32)
    tmpb = sb("tmpb", [128, 4, 64], F32)
    maddT = sb("maddT", [128, 2, 4, 128], BF16)
    accg = sb("accg", [128, 2, 4, 64], F32)
    S.op("dve", lambda e: e.memset(eps_t[:, :], EPS), writes=["eps"])
    S.op("dve", lambda e: e.memset(selb[:, :], 0.0), writes=["selb"])
    S.dma("pool", wq[:, :, :], wq_d[:, :, :], writes=["wq"])
    S.op("pool", lambda e: e.memset(Vd[:, :, :, 64:65], 1.0), writes=["Vd1"])
    S.op("pool", lambda e: e.memset(VS[:, :, :, 64:65], 1.0), writes=["VS1"])
    S.op("dve", lambda e: e.memset(run[:, :], 0.0), writes=["run"])
    S.op("dve", lambda e: e.memset(QZd[:, :, :], 0.0), writes=["QZ"])
    S.op("dve", lambda e: e.memset(QZn[:, :, :, :], 0.0), writes=["QZ"])
    S.op("dve", lambda e: e.memset(maddT[:, :, :, :], 0.0), writes=["madd0", "madd1"])
    if stop_after is None:
        zsrc = mk_ap(maddT[:, 0, 0, 0:1], [[0, 8], [1, 1024]])
        for zi in range(NSLOT * SROWS // 1024):
            S.dma("sp" if zi % 2 == 0 else "act", hs_d[zi * 1024:(zi + 1) * 1024, :].rearrange("(i p) d -> p i d", p=128), zsrc,
                  reads=["madd0", "madd1"])
    S.op("dve", lambda e: e.memset(VCX[:, :, 64:65], 1.0), writes=["VCX1"])
    for g in range(2):
        S.op("dve", lambda e: e.tensor_copy(out=VCX[0:127, g, 65:97], in_=cf[0:127, CF_OV:CF_OV + 32]), reads=["cf"], writes=["VCX2"])
    S.barrier()
    psflat = ps[:, :, :].rearrange("p b n -> p (b n)")

    def rstd(ss_ap, out_ap, kin, kout):
        S.op("act", lambda e: e.activation(out=out_ap, in_=ss_ap, func=AF.Ln, scale=1.0 / D, bias=eps_t[:, 0:1]),
             reads=[kin, "eps"], writes=[kout])
        S.op("act", lambda e: e.activation(out=out_ap, in_=out_ap, func=AF.Exp, scale=-0.5), reads=[kout], writes=[kout])

    def rope(src3, dst3, nh, cs, sn, npart, ri, src_keys, dst_keys):
        r = rt[ri]
        x1 = src3[:, :, 0:8]
        x2 = src3[:, :, 8:16]
        tmp = lambda j: r[0:npart, j, 0:nh * 8].rearrange("p (h e) -> p h e", h=nh)
        rk = f"rt{ri}"
        for j, (xx, tb) in enumerate([(x1, cs), (x2, sn), (x2, cs), (x1, sn)]):
            S.op("dve", lambda e: e.tensor_tensor(out=tmp(j), in0=xx, in1=tb, op=ALU.mult), reads=src_keys + ["cf"], writes=[rk])
        S.op("dve", lambda e: e.tensor_tensor(out=dst3[:, :, 0:8], in0=tmp(0), in1=tmp(1), op=ALU.subtract), reads=[rk], writes=dst_keys)
        S.op("dve", lambda e: e.tensor_tensor(out=dst3[:, :, 8:16], in0=tmp(2), in1=tmp(3), op=ALU.add), reads=[rk], writes=dst_keys)
        S.op("act", lambda e: e.copy(out=dst3[:, :, 16:64], in_=src3[:, :, 16:64]), reads=src_keys, writes=dst_keys)

    def tab(off, i, nh, npart=128):
        return mk_ap(cf[0:npart, off + i * 8:off + i * 8 + 1], [[0, nh], [1, 8]])

    def transposes(src2d, bank, nblk, npart, src_keys):
        for c in range(nblk):
            S.op("pe", lambda e: e.transpose(out=psb(bank)[:, c * 128:c * 128 + npart], in_=src2d[0:npart, c * 128:(c + 1) * 128],
                                             identity=cb[0:npart, CB_ID:CB_ID + npart]),
                 reads=src_keys + ["cb"], writes=[PSK(bank)], inc=(c == nblk - 1))

    bc_reg = nc.gpsimd.alloc_register("bcreg")
    nc.gpsimd.reg_mov(bc_reg, NE * 128 - 1)
    for s in range(nseq):
        S.dma("pool", wkv[:, :, :], wkv_d[:, :, :], writes=["wkv"])
        S.dma("sp", g1b[:, :], g1_d.partition_broadcast(128), writes=["g1b"])

        def pa_front(i):
            k = i % 2
            tsl = slice(i * 128, (i + 1) * 128)
            S.dma("sp", xt[k][:, :], x_d[s, tsl, :], writes=[f"xt{k}"])
            S.op("act", lambda e: e.activation(out=xn[k][:, :], in_=xt[k][:, :], func=AF.Square, accum_out=st[k][:, 0:1]),
                 reads=[f"xt{k}"], writes=[f"xn{k}", f"ss{k}"])
            rstd(st[k][:, 0:1], st[k][:, 1:2], f"ss{k}", f"rstd{k}")
            S.op("dve", lambda e: e.scalar_tensor_tensor(out=xn[k][:, :], in0=xt[k][:, :], scalar=st[k][:, 1:2], in1=g1b[:, :],
                                                         op0=ALU.mult, op1=ALU.mult),
                 reads=[f"xt{k}", f"rstd{k}", "g1b"], writes=[f"xn{k}"])
            transposes(xn[k], 7, 8, 128, [f"xn{k}"])
            S.op("dve", lambda e: e.tensor_copy(out=xT[:, :, tsl], in_=psb(7).rearrange("p (c n) -> p c n", c=8)),
                 reads=[PSK(7)], writes=[f"xT{i}"])

        def pa_proj(i):
            tsl = slice(i * 128, (i + 1) * 128)
            for nb, (n0, n1) in enumerate([(0, 512), (512, 1024), (1024, 1536), (1536, 1792)]):
                for c in range(8):
                    S.op("pe", lambda e: e.matmul(ps[:, nb, 0:n1 - n0], lhsT=xT[:, c, tsl], rhs=wkv[:, c, n0:n1], start=(c == 0), stop=(c == 7)),
                         reads=[f"xT{i}", "wkv"], writes=[PSK(nb)], inc=(c == 7))

        def pa_evac(i):
            k = i % 2
            src3 = psflat[:, 0:768].rearrange("p (h e) -> p h e", h=12)
            dst3 = ktm[k][:, 0:768].rearrange("p (h e) -> p h e", h=12)
            rope(src3, dst3, 12, tab(CF_COS, i, 12), tab(CF_SIN, i, 12), 128, k, [PSK(0), PSK(1)], [f"ktm{k}"])
            S.op("act", lambda e: e.copy(out=ktm[k][:, 768:1024], in_=psflat[:, 768:1024]), reads=[PSK(1)], writes=[f"ktm{k}"])
            S.op("act", lambda e: e.copy(out=Vd[:, i, :, 0:64], in_=ps[:, 2, :].rearrange("p (h e) -> p h e", h=8)),
                 reads=[PSK(2)], writes=["Vd"])
            S.op("act", lambda e: e.copy(out=VS[:, i, :, 0:64], in_=ps[:, 3, 0:256].rearrange("p (h e) -> p h e", h=4)),
                 reads=[PSK(3)], writes=["VS"])

        def pa_back(i):
            k = i % 2
            tsl = slice(i * 128, (i + 1) * 128)
            transposes(ktm[k], 6, 8, 128, [f"ktm{k}"])
            S.op("dve", lambda e: e.tensor_copy(out=KT[:, :, tsl], in_=psb(6).rearrange("p (c n) -> p c n", c=8)),
                 reads=[PSK(6)], writes=["KT"])

        pa_front(0)
        pa_proj(0)
        for i in range(NT):
            if i + 1 < NT:
                pa_front(i + 1)
            pa_evac(i)
            if i + 1 < NT:
                pa_proj(i + 1)
            pa_back(i)
        S.barrier()
        if stop_after == "A":
            break
        S.dma("sp", g2b[:, :], g2_d.partition_broadcast(128), writes=["g2b"])
        for kv in range(2):
            S.dma("pool", w1[:, :, :], w1_d[kv], writes=["w1"])
            zin = mk_ap(KT[:, 6 + kv, 0:1], [[16, 127], [1, 32]])
            pin = mk_ap(peT[:, kv, 0:1], [[0, 127], [1, 32]])
            S.op("dve", lambda e: e.tensor_tensor(out=mk_ap(Z[:, 0, 0:1], [[1, 127], [128, 32]]), in0=zin, in1=pin, op=ALU.add), reads=["KT", "peT"], writes=["Z"])
            if CUT < 2:
                continue
            KVAR = int(os.environ.get("KVAR", "0"))
            for g in ([0] if KVAR == 1 else [1] if KVAR == 4 else range(2)):
                for hc in range(2):
                    col = hc * 128
                    nq = 8 if KVAR == 2 else 32
                    for q in range(nq):
                        S.op("pe", lambda e: e.matmul(ps[:, g, col:col + 128], lhsT=w1[g * 64:(g + 1) * 64, q, hc * 128:(hc + 1) * 128],
                                                      rhs=Z[g * 64:(g + 1) * 64, q, 0:128], start=(q == 0), stop=(q == nq - 1)),
                             reads=["w1", "Z"], writes=[PSK(g)], inc=(q == nq - 1 or KVAR == 3))
            if CUT < 3:
                continue
            xs = xt[0][:, 0:512]
            uu = xt[0][:, 512:1024]
            for g in range(2):
                S.op("act", lambda e: e.copy(out=xs[:, g * 256:(g + 1) * 256], in_=ps[:, g, 0:256]), reads=[PSK(g)], writes=["xs"])
            S.op("dve", lambda e: e.tensor_tensor(out=uu, in0=xs, in1=xs, op=ALU.mult), reads=["xs"], writes=["uu"])
            S.op("dve", lambda e: e.tensor_scalar(out=uu, in0=uu, scalar1=0.044715, scalar2=1.0, op0=ALU.mult, op1=ALU.add), reads=["uu"], writes=["uu"])
            S.op("dve", lambda e: e.tensor_tensor(out=uu, in0=uu, in1=xs, op=ALU.mult), reads=["uu", "xs"], writes=["uu"])
            S.op("act", lambda e: e.activation(out=uu, in_=uu, func=AF.Tanh, scale=0.7978845608), reads=["uu"], writes=["uu"])
            S.op("dve", lambda e: e.scalar_tensor_tensor(out=uu, in0=uu, scalar=1.0, in1=xs, op0=ALU.add, op1=ALU.mult), reads=["uu", "xs"], writes=["uu"])
            S.op("act", lambda e: e.mul(out=Pb[0][:, :], in_=uu, mul=0.5), reads=["uu"], writes=["P0"])
            if CUT < 4:
                continue
            for g in range(2):
                for hc in range(2):
                    col = (g * 2 + hc) * 128
                    S.op("pe", lambda e: e.matmul(ps[0:127, 2, g * 64:(g + 1) * 64], lhsT=Pb[0][:, col:col + 127], rhs=w2[:, kv, hc, :],
                                                  start=(hc == 0), stop=(hc == 1)),
                         reads=["P0", "w2"], writes=[PSK(2)], inc=(hc == 1))
            if CUT < 5:
                continue
            src3 = ps[0:127, 2, 0:128].rearrange("p (h e) -> p h e", h=2)
            if kv == 0:
                dst3 = ktm[0][0:127, 0:128].rearrange("p (h e) -> p h e", h=2)
                rope(src3, dst3, 2, tab(CF_COSC, 0, 2, 127), tab(CF_SINC, 0, 2, 127), 127, 0, [PSK(2)], ["ktm0"])
                transposes(ktm[0], 6, 1, 127, ["ktm0"])
                S.op("dve", lambda e: e.tensor_copy(out=KCT[:, 0:127], in_=psb(6)[:, 0:127]), reads=[PSK(6)], writes=["KCT"])
            else:
                S.op("act", lambda e: e.copy(out=VCX[0:127, :, 0:64], in_=src3), reads=[PSK(2)], writes=["VCX"])
        S.barrier()
        if stop_after == "C":
            break
        S.dma("pool", wo[:, :, :], wo_d[:, :, :], writes=["wo"])
        def make_tile(t, pre_stages=()):
                qk = t % 2
                tsl = slice(t * 128, (t + 1) * 128)
                for (n0, n1, bank) in [(0, 512, 4), (512, 1024, 5), (1024, 1048, 3)]:
                    for c in range(8):
                        S.op("pe", lambda e: e.matmul(ps[:, bank, 0:n1 - n0], lhsT=xT[:, c, tsl], rhs=wq[:, c, n0:n1], start=(c == 0), stop=(c == 7)),
                             reads=[f"xT{t}", "wq"], writes=[PSK(bank)], inc=(c == 7))
                for pst in pre_stages:
                    pst()
                src3 = psflat[:, 2048:3072].rearrange("p (h e) -> p h e", h=16)
                dst3 = ktm[qk][:, :].rearrange("p (h e) -> p h e", h=16)
                rope(src3, dst3, 16, tab(CF_COS, t, 16), tab(CF_SIN, t, 16), 128, qk, [PSK(4), PSK(5)], [f"ktm{qk}"])
                S.op("act", lambda e: e.activation(out=gate[:, :], in_=ps[:, 3, 0:24], func=AF.Exp, scale=-1.0), reads=[PSK(3)], writes=["gate"])
                S.op("dve", lambda e: e.tensor_scalar_add(out=gate[:, :], in0=gate[:, :], scalar1=1.0), reads=["gate"], writes=["gate"])
                S.op("dve", lambda e: e.reciprocal(out=gate[:, :], in_=gate[:, :]), reads=["gate"], writes=["gate"])
                transposes(ktm[qk], 7, 8, 128, [f"ktm{qk}"])
                p7 = psb(7).rearrange("p (c n) -> p c n", c=8)
                QZv = QZd[:, :, :].rearrange("p (b two) n -> p b two n", two=2)
                S.op("dve", lambda e: e.tensor_copy(out=QZv[0:64, :, 0, :], in_=p7[0:64, 0:4, :]), reads=[PSK(7)], writes=["QZ"])
                S.op("act", lambda e: e.copy(out=QZv[64:128, :, 1, :], in_=p7[64:128, 0:4, :]), reads=[PSK(7)], writes=["QZ"])
                S.op("dve", lambda e: e.tensor_copy(out=QZn[0:64, 0, :, :], in_=p7[0:64, 4:8, :]), reads=[PSK(7)], writes=["QZ"])
                S.op("act", lambda e: e.copy(out=QZn[64:128, 1, :, :], in_=p7[64:128, 4:8, :]), reads=[PSK(7)], writes=["QZ"])
                QK_ = "QZ"
                units = []

                def gate_ap(g, b):
                    return mk_ap(gate[:, g * 12 + b:g * 12 + b + 1], [[3, 4]])

                def bc4(ap2):
                    return mk_ap(ap2, [[ap2.ap[1][0], 4], [0, 64]])

                def nsa_fin(g, bank, w, branch, first):
                    O3 = ps[:, bank, 0:4 * w].rearrange("p (r e) -> p r e", r=4)
                    S.op("dve", lambda e: e.tensor_scalar_max(out=sm[:, 4:8], in0=O3[:, :, 64], scalar1=1e-30), reads=[PSK(bank)], writes=["sm4"])
                    S.op("dve", lambda e: e.reciprocal(out=sm[:, 8:12], in_=sm[:, 4:8]), reads=["sm4"], writes=["sm8"])
                    if branch == 0:
                        S.op("dve", lambda e: e.tensor_tensor(out=imp[:, :, :], in0=O3[:, :, 65:97],
                                                              in1=mk_ap(sm[:, 8:9], [[1, 4], [0, 32]]), op=ALU.mult),
                             reads=[PSK(bank), "sm8"], writes=["imp"])
                    S.op("dve", lambda e: e.tensor_tensor(out=sm[:, 12:16], in0=sm[:, 8:12], in1=gate_ap(g, branch), op=ALU.mult),
                         reads=["sm8", "gate"], writes=["sm12"])
                    fb = mk_ap(sm[:, 12:13], [[1, 4], [0, 64]])
                    if first:
                        S.op("dve", lambda e: e.tensor_tensor(out=accg[:, g, :, :], in0=O3[:, :, 0:64], in1=fb, op=ALU.mult),
                             reads=[PSK(bank), "sm12"], writes=[f"accg{g}"])
                    else:
                        S.op("dve", lambda e: e.tensor_tensor(out=tmpb[:, :, :], in0=O3[:, :, 0:64], in1=fb, op=ALU.mult),
                             reads=[PSK(bank), "sm12"], writes=["tmpb"])
                        S.op("dve", lambda e: e.tensor_tensor(out=accg[:, g, :, :], in0=accg[:, g, :, :], in1=tmpb[:, :, :], op=ALU.add),
                             reads=["tmpb", f"accg{g}"], writes=[f"accg{g}"])

                def select(g):
                    S.op("dve", lambda e: e.tensor_reduce(out=rs[:, 0:32], in_=imp[:, :, :].rearrange("p r j -> p j r"), axis=AX.X, op=ALU.add),
                         reads=["imp"], writes=["rs0"])
                    S.op("dve", lambda e: e.tensor_tensor(out=rs[:, 0:32], in0=rs[:, 0:32], in1=cf[:, CF_SA + t * 32:CF_SA + (t + 1) * 32], op=ALU.mult),
                         reads=["rs0", "cf"], writes=["rs0"])
                    S.op("dve", lambda e: e.tensor_tensor(out=rs[:, 0:32], in0=rs[:, 0:32], in1=cf[:, CF_SB + t * 32:CF_SB + (t + 1) * 32], op=ALU.add),
                         reads=["rs0", "cf"], writes=["rs0"])
                    S.op("dve", lambda e: e.max(out=rs[:, 32:40], in_=rs[:, 0:32]), reads=["rs0"], writes=["rs32"])
                    pg_ = g * 64
                    S.op("dve", lambda e: e.tensor_scalar(out=selb[:, pg_:pg_ + 32], in0=rs[:, 0:32], scalar1=rs[:, 39:40], scalar2=NEGM, op0=ALU.is_lt, op1=ALU.mult),
                         reads=["rs0", "rs32"], writes=["selb"])

                def select_b(g):
                    pg_ = g * 64
                    S.op("pe", lambda e: e.transpose(out=psb(7)[0:96, 0:128], in_=selb[:, 0:96], identity=ident), reads=["selb", "cb"], writes=[PSK(7)])
                    S.op("dve", lambda e: e.tensor_copy(out=maddT[pg_:pg_ + 32, g, :, :], in_=mk_ap(psb(7)[pg_:pg_ + 32, 0:1], [[0, 4], [1, 128]])),
                         reads=[PSK(7)], writes=[f"madd{g}"])

                def add_unit(qkf, postf, pvf, hook=None):
                    units.append((qkf, postf, pvf, hook))

                def mk_dil(h, kg, nb, first, last, Ob, slot):
                    hp, pr = h // 2, (h % 2) * 64
                    def qkf(stb):
                        for b in range(nb):
                            S.op("pe", lambda e: e.matmul(ps[:, stb, b * 128:(b + 1) * 128], lhsT=KT[:, hp, (kg + b) * 128:(kg + b + 1) * 128],
                                                          rhs=QZd[:, h, :], start=True, stop=True),
                                 reads=["KT", QK_], writes=[PSK(stb)], inc=(b == nb - 1))
                    def postf(stb, pi):
                        P = Pb[pi]
                        S.op("act", lambda e: e.activation(out=P[:, 0:nb * 128], in_=ps[:, stb, 0:nb * 128], func=AF.Exp, scale=0.125),
                             reads=[PSK(stb)], writes=[f"P{pi}"])
                        m0 = 15 - t + kg
                        S.op("dve", lambda e: e.tensor_tensor(out=P[:, 0:nb * 128], in0=P[:, 0:nb * 128],
                                                              in1=cb[:, CB_MALL + m0 * 128:CB_MALL + (m0 + nb) * 128], op=ALU.mult),
                             reads=[f"P{pi}", "cb"], writes=[f"P{pi}"])
                    def pvf(pi):
                        P = Pb[pi]
                        for b in range(nb):
                            S.op("pe", lambda e: e.matmul(ps[:, Ob, slot * 65:(slot + 1) * 65], lhsT=P[:, b * 128:(b + 1) * 128], rhs=Vd[:, kg + b, h, :],
                                                          start=(first and b == 0), stop=(last and b == nb - 1), skip_group_check=True),
                                 reads=[f"P{pi}", "Vd"], writes=[PSK(Ob)], inc=(b == nb - 1))
                    return qkf, postf, pvf

                def dil_fin(quad, Ob):
                    O3 = ps[:, Ob, 0:260].rearrange("p (r e) -> p r e", r=4)
                    S.op("dve", lambda e: e.reciprocal(out=sm[:, 0:4], in_=O3[:, :, 64]), reads=[PSK(Ob)], writes=["sm0"])
                    S.op("dve", lambda e: e.tensor_tensor(out=mixb[:, quad * 256:(quad + 1) * 256].rearrange("p (r e) -> p r e", r=4),
                                                          in0=O3[:, :, 0:64], in1=mk_ap(sm[:, 0:1], [[1, 4], [0, 64]]), op=ALU.mult),
                         reads=[PSK(Ob), "sm0"], writes=[f"ktm{qk}"])

                def mk_nsa(g, kind, j, first, last, Ob):
                    pg = g * 64
                    kp = 127 if kind == "cmp" else 128
                    w = 97 if kind == "cmp" else 65
                    def qkf(stb):
                        if kind == "cmp":
                            lhs = KCT[:, 0:127]
                        else:
                            lhs = KT[:, 4 if kind == "sel" else 5, j * 128:(j + 1) * 128]
                        S.op("pe", lambda e: e.matmul(ps[0:kp, stb, 0:512], lhsT=lhs, rhs=QZn[:, g, :, :].rearrange("p r q -> p (r q)"),
                                                      start=True, stop=(kind != "sel"), skip_group_check=True),
                             reads=["KT", "KCT", QK_], writes=[PSK(stb)], inc=(kind != "sel"))
                        if kind == "sel":
                            S.op("pe", lambda e: e.matmul(ps[:, stb, 0:512], lhsT=cb[:, CB_E + j * 128:CB_E + (j + 1) * 128],
                                                          rhs=maddT[:, g, :, :].rearrange("p r q -> p (r q)"), start=False, stop=True, skip_group_check=True),
                                 reads=["cb", f"madd{g}"], writes=[PSK(stb)])
                    def postf(stb, pi):
                        P = Pb[pi]
                        S.op("act", lambda e: e.activation(out=P[0:kp, :], in_=ps[0:kp, stb, :], func=AF.Exp, scale=0.125),
                             reads=[PSK(stb)], writes=[f"P{pi}"])
                        P3 = P[0:kp, :].rearrange("p (r q) -> p r q", r=4)
                        msk = None
                        if kind == "cmp":
                            msk = mk_ap(cb[0:127, CB_CM + t * 128:CB_CM + t * 128 + 1], [[0, 4], [1, 128]])
                        elif j == t:
                            msk = mk_ap(cb[:, CB_TRI:CB_TRI + 1], [[0, 4], [1, 128]])
                        elif kind == "win" and j == t - 4:
                            msk = mk_ap(cb[:, CB_TRIW:CB_TRIW + 1], [[0, 4], [1, 128]])
                        if msk is not None:
                            S.op("dve", lambda e: e.tensor_tensor(out=P3, in0=P3, in1=msk, op=ALU.mult), reads=[f"P{pi}", "cb"], writes=[f"P{pi}"])
                    def pvf(pi):
                        P = Pb[pi]
                        for r in range(4):
                            if kind == "cmp":
                                rhs = VCX[0:127, g, :]
                            else:
                                rhs = VS[:, j, (0 if kind == "sel" else 2) + g, :]
                            S.op("pe", lambda e: e.matmul(ps[:, Ob, r * w:(r + 1) * w], lhsT=P[0:kp, r * 128:(r + 1) * 128], rhs=rhs,
                                                          start=(first and r == 0), stop=(last and r == 3), skip_group_check=True),
                                 reads=[f"P{pi}", "VS", "VCX"], writes=[PSK(Ob)], inc=(r == 3))
                    return qkf, postf, pvf

                for g in range(2):
                    add_unit(*mk_nsa(g, "cmp", 0, True, True, 4 + g),
                             hook=(lambda g=g: (nsa_fin(g, 4 + g, 97, 0, True), select(g), [(4, (lambda g=g: select_b(g)))])[2]))
                for quad in range(2):
                    Ob = 3
                    kgs = list(range(0, t + 1, 4))
                    for hi in range(4):
                        h = quad * 4 + hi
                        for ki, kg in enumerate(kgs):
                            nb = min(4, t + 1 - kg)
                            lastu = (ki == len(kgs) - 1)
                            hk = (lambda quad=quad, Ob=Ob: dil_fin(quad, Ob)) if (hi == 3 and lastu) else None
                            add_unit(*mk_dil(h, kg, nb, ki == 0, lastu, Ob, hi), hook=hk)
                for g in range(2):
                    for j in range(0, t + 1):
                        hk = (lambda g=g: nsa_fin(g, 4 + g, 65, 1, False)) if j == t else None
                        add_unit(*mk_nsa(g, "sel", j, j == 0, j == t, 4 + g), hook=hk)
                for g in range(2):
                    j0 = max(0, t - 4)
                    for j in range(j0, t + 1):
                        def hk_win(g=g):
                            nsa_fin(g, 4 + g, 65, 2, False)
                            S.op("act", lambda e: e.copy(out=mixb[:, 512 + g * 256:512 + (g + 1) * 256].rearrange("p (r e) -> p r e", r=4), in_=accg[:, g, :, :]),
                                 reads=[f"accg{g}"], writes=[f"ktm{qk}"])
                        add_unit(*mk_nsa(g, "win", j, j == j0, j == t, 4 + g), hook=(hk_win if j == t else None))
                stages = []
                R = lambda a, b: rs[:, a:b]
                dv = lambda f, rd, wr_: S.op("dve", f, reads=rd, writes=wr_)
                mixb = ktm[qk]
                def stage(f):
                    stages.append(f)
                    return f
                @stage
                def _s0():
                    transposes(mixb, 7, 8, 128, [f"ktm{qk}"])
                    S.op("dve", lambda e: e.tensor_copy(out=mixT[:, :, :], in_=psb(7).rearrange("p (c n) -> p c n", c=8)), reads=[PSK(7)], writes=["mixT"])
                @stage
                def _s1():
                    S.dma("sp", xt[qk][:, :], x_d[s, tsl, :], writes=[f"xt{qk}"])
                    for half in range(2):
                        hs_ = slice(half * 512, (half + 1) * 512)
                        for c in range(8):
                            S.op("pe", lambda e: e.matmul(ps[:, 7, :], lhsT=mixT[:, c, :], rhs=wo[:, c, hs_], start=(c == 0), stop=(c == 7)),
                                 reads=["mixT", "wo"], writes=[PSK(7)], inc=(c == 7))
                        S.op("dve", lambda e: e.tensor_tensor(out=xm[:, hs_], in0=ps[:, 7, :], in1=xt[qk][:, hs_], op=ALU.add),
                             reads=[PSK(7), f"xt{qk}"], writes=["xm"])
                @stage
                def _s2():
                    S.dma("sp", xmid_d[s * S_LEN + t * 128:s * S_LEN + (t + 1) * 128, :], xm[:, :], reads=["xm"])
                    S.op("act", lambda e: e.activation(out=h2[:, :], in_=xm[:, :], func=AF.Square, accum_out=st[qk][:, 2:3]),
                         reads=["xm"], writes=["h2", f"ssb{qk}"])
                    rstd(st[qk][:, 2:3], st[qk][:, 3:4], f"ssb{qk}", f"rstdb{qk}")
                    S.op("dve", lambda e: e.scalar_tensor_tensor(out=h2[:, :], in0=xm[:, :], scalar=st[qk][:, 3:4], in1=g2b[:, :], op0=ALU.mult, op1=ALU.mult),
                         reads=["xm", f"rstdb{qk}", "g2b"], writes=["h2"])
                    S.dma("sp", h2s_d[s * S_LEN + t * 128:s * S_LEN + (t + 1) * 128, :], h2[:, :], reads=["h2"])
                @stage
                def _s3():
                    transposes(h2, 7, 8, 128, ["h2"])
                    S.op("dve", lambda e: e.tensor_copy(out=mixT[:, :, :], in_=psb(7).rearrange("p (c n) -> p c n", c=8)), reads=[PSK(7)], writes=["mixT"])
                    for c in range(8):
                        S.op("pe", lambda e: e.matmul(ps[:, 7, 0:72], lhsT=mixT[:, c, :], rhs=wr[:, c, :], start=(c == 0), stop=(c == 7)),
                             reads=["mixT", "wr"], writes=[PSK(7)], inc=(c == 7))
                @stage
                def _s4():
                    dv(lambda e: e.tensor_tensor(out=R(64, 136), in0=ps[:, 7, 0:72], in1=brb[:, :], op=ALU.add), [PSK(7), "brb"], ["lg"])
                    dv(lambda e: e.max(out=R(136, 144), in_=R(64, 72)), ["lg"], ["mg"])
                    dv(lambda e: e.tensor_scalar(out=R(144, 152), in0=R(64, 72), scalar1=R(136, 137), scalar2=None, op0=ALU.is_ge), ["lg", "mg"], ["ohg"])
                    dv(lambda e: e.tensor_scalar_mul(out=R(152, 153), in0=R(136, 137), scalar1=-1.0), ["mg"], ["nmg"])
                    S.op("act", lambda e: e.activation(out=R(160, 168), in_=R(64, 72), func=AF.Exp, bias=R(152, 153), accum_out=R(153, 154)),
                         reads=["lg", "nmg"], writes=["eg", "sg"])
                    dv(lambda e: e.reciprocal(out=R(154, 155), in_=R(153, 154)), ["sg"], ["pg"])
                @stage
                def _s5():
                    el3 = R(72, 136).rearrange("p (g x) -> p g x", g=8)
                    dv(lambda e: e.tensor_tensor(out=R(168, 232).rearrange("p (g x) -> p g x", g=8), in0=el3, in1=mk_ap(R(144, 145), [[1, 8], [0, 8]]), op=ALU.mult),
                       ["lg", "ohg"], ["elm"])
                    dv(lambda e: e.tensor_reduce(out=R(232, 240), in_=R(168, 232).rearrange("p (g x) -> p x g", g=8), axis=AX.X, op=ALU.add), ["elm"], ["els"])
                    dv(lambda e: e.max(out=R(240, 248), in_=R(232, 240)), ["els"], ["m8"])
                    dv(lambda e: e.tensor_tensor(out=R(248, 249), in0=R(241, 242), in1=R(240, 241), op=ALU.subtract), ["m8"], ["dd"])
                    S.op("act", lambda e: e.activation(out=R(249, 250), in_=R(248, 249), func=AF.Exp), reads=["dd"], writes=["r21"])
                    dv(lambda e: e.tensor_scalar_add(out=R(250, 251), in0=R(249, 250), scalar1=1.0), ["r21"], ["den"])
                    dv(lambda e: e.reciprocal(out=R(251, 252), in_=R(250, 251)), ["den"], ["rden"])
                    dv(lambda e: e.tensor_tensor(out=R(252, 253), in0=R(251, 252), in1=R(154, 155), op=ALU.mult), ["rden", "pg"], ["w1v"])
                    dv(lambda e: e.tensor_tensor(out=R(253, 254), in0=R(252, 253), in1=R(249, 250), op=ALU.mult), ["w1v", "r21"], ["w2v"])
                    dv(lambda e: e.tensor_copy(out=WV[:, s, t:t + 1], in_=R(252, 253)), ["w1v"], ["WV"])
                    dv(lambda e: e.tensor_copy(out=WV[:, s, 16 + t:17 + t], in_=R(253, 254)), ["w2v"], ["WV"])
                @stage
                def _s6():
                    for ch in range(2):
                        dv(lambda e: e.tensor_scalar(out=R(40 + ch * 8, 48 + ch * 8), in0=R(232, 240), scalar1=R(240 + ch, 241 + ch), scalar2=None, op0=ALU.is_equal),
                           ["els", "m8", "rs0"], [f"m{ch}"])
                        dv(lambda e: e.tensor_tensor(out=OH[:, ch, t, :].rearrange("p (g x) -> p g x", g=8), in0=mk_ap(R(144, 145), [[1, 8], [0, 8]]),
                                                     in1=mk_ap(R(40 + ch * 8, 41 + ch * 8), [[0, 8], [1, 8]]), op=ALU.mult), ["ohg", f"m{ch}"], [f"OH{t}"])
                    UT = cb[:, CB_UT:CB_UT + 128]
                    ON = cb[:, CB_ONES:CB_ONES + 128]
                    pm = lambda o0, lh, ch, st_, sp_: S.op("pe", lambda e: e.matmul(ps[:, 7, 128 + o0:128 + o0 + 64], lhsT=lh, rhs=OH[:, ch, t, :], start=st_, stop=sp_, skip_group_check=True),
                                                          reads=[f"OH{t}", "cb"], writes=[PSK(7)], inc=sp_)
                    pm(0, UT, 0, True, True)
                    pm(64, UT, 1, True, False)
                    pm(64, ON, 0, False, True)
                    pm(128, ON, 0, True, False)
                    pm(128, ON, 1, False, True)
                @stage
                def _s7():
                    tm2 = tmpb[:, :, :].rearrange("p r e -> p (r e)")[:, 0:128].rearrange("p (c e) -> p c e", c=2)
                    dv(lambda e: e.tensor_tensor(out=tm2, in0=ps[:, 7, 128:256].rearrange("p (c e) -> p c e", c=2), in1=mk_ap(run[:, 0:1], [[0, 2], [1, 64]]), op=ALU.add),
                       [PSK(7), "run"], ["tmpb"])
                    dv(lambda e: e.tensor_tensor(out=tm2, in0=tm2, in1=mk_ap(OH[:, 0, t, 0:1], [[NT * NE, 2], [1, 64]]), op=ALU.mult), ["tmpb", f"OH{t}"], ["tmpb"])
                    dv(lambda e: e.tensor_reduce(out=mk_ap(RK[:, s, t:t + 1], [[16, 2]]), in_=tm2, axis=AX.X, op=ALU.add), ["tmpb"], ["RK"])
                    dv(lambda e: e.tensor_tensor(out=run[:, :], in0=run[:, :], in1=ps[:, 7, 256:320], op=ALU.add), [PSK(7), "run"], ["run"])
                return units, stages

        prev_stages = []
        for t in range(NT):
            NPRE = int(os.environ.get('NPRE', '0'))
            units, stages_t = make_tile(t, prev_stages[0:NPRE])
            prev_stages = prev_stages[NPRE:]
            nst = len(prev_stages)
            gap = max(1, len(units) // (nst + 1)) if nst else 0
            si = 0
            deferred = []
            LA = 3
            STB = [0, 1, 2, 6]
            for ui, (qkf, postf, pvf, hook) in enumerate(units):
                if ui == 0:
                    for la in range(min(LA, len(units))):
                        units[la][0](STB[la % 4])
                if ui + LA < len(units):
                    units[ui + LA][0](STB[(ui + LA) % 4])
                postf(STB[ui % 4], ui % 4)
                pvf(ui % 4)
                if hook is not None:
                    r_ = hook()
                    if isinstance(r_, list):
                        for (dl_, fn_) in r_:
                            deferred.append((ui + dl_, fn_))
                for (du_, fn_) in [d_ for d_ in deferred if d_[0] <= ui]:
                    fn_()
                deferred = [d_ for d_ in deferred if d_[0] > ui]
                if nst and (ui + 1) % gap == 0 and si < nst:
                    prev_stages[si]()
                    si += 1
            for (du_, fn_) in deferred:
                fn_()
            while si < nst:
                prev_stages[si]()
                si += 1
            prev_stages = stages_t
        for st_ in prev_stages:
            st_()
        S.barrier()
        S.dma("sp", oh_d[s], OH[:, :, :, :].rearrange("p c t e -> p (c t e)"), reads=[])
        S.barrier()
        if stop_after == "B":
            break
    dv = lambda f, rd, wr_: S.op("dve", f, reads=rd, writes=wr_)
    scrA = R1[:, 0:8192].bitcast(F32)
    scr = scrA[:, 0:1024].rearrange("p (a b) -> p a b", a=16)
    dv(lambda e: e.tensor_tensor(out=scrA.rearrange("p (a b) -> p a b", a=64), in0=mk_ap(run[:, 0:1], [[1, 64], [0, 32]]),
                                 in1=mk_ap(cf[:, CF_THR:CF_THR + 1], [[0, 64], [1, 32]]), op=ALU.is_gt), ["run", "cf"], ["scr"])
    dv(lambda e: e.tensor_reduce(out=ntl[:, :], in_=scrA.rearrange("p (a b) -> p a b", a=64), axis=AX.X, op=ALU.add), ["scr"], ["ntl"])
    dv(lambda e: e.tensor_copy(out=cum[0][:, :], in_=ntl[:, :]), ["ntl"], ["cum0"])
    cur = 0
    for sh in (1, 2, 4, 8, 16, 32):
        a_, b_ = cum[cur], cum[1 - cur]
        dv(lambda e: e.tensor_copy(out=b_[:, 0:sh], in_=a_[:, 0:sh]), [f"cum{cur}"], [f"cum{1 - cur}"])
        dv(lambda e: e.tensor_tensor(out=b_[:, sh:64], in0=a_[:, sh:64], in1=a_[:, 0:64 - sh], op=ALU.add), [f"cum{cur}"], [f"cum{1 - cur}"])
        cur = 1 - cur
    cinc = cum[cur]
    sbase = cum[1 - cur]
    dv(lambda e: e.tensor_tensor(out=sbase[:, :], in0=cinc[:, :], in1=ntl[:, :], op=ALU.subtract), [f"cum{cur}", "ntl"], [f"cum{1 - cur}"])
    dv(lambda e: e.tensor_scalar_mul(out=sbase[:, :], in0=sbase[:, :], scalar1=float(SROWS)), [f"cum{1 - cur}"], [f"cum{1 - cur}"])
    for s in range(nseq):
        S.dma("sp", OH[:, :, :, :].rearrange("p c t e -> p (c t e)"), oh_d[s], writes=["OH"])
        for ch in range(2):
            dv(lambda e: e.tensor_tensor(out=scr, in0=OH[:, ch, :, :], in1=mk_ap(sbase[:, 0:1], [[0, 16], [1, 64]]), op=ALU.mult),
               ["OH", f"cum{1 - cur}"], ["scr"])
            dv(lambda e: e.tensor_reduce(out=EF[:, 0:16], in_=scr, axis=AX.X, op=ALU.add), ["scr"], ["EF"])
            dv(lambda e: e.tensor_tensor(out=EF[:, 16:32], in0=EF[:, 0:16], in1=RK[:, s, ch * 16:(ch + 1) * 16], op=ALU.add), ["EF", "RK"], ["EF"])
            dv(lambda e: e.tensor_copy(out=DESTI[:, s, ch * 16:(ch + 1) * 16], in_=EF[:, 16:32]), ["EF"], ["DESTI"])
    for sc in range(NSLOT // 16):
        dv(lambda e: e.tensor_tensor(out=scr, in0=mk_ap(cinc[:, 0:1], [[0, 16], [1, 64]]),
                                     in1=mk_ap(cf[:, CF_SIDX + sc * 16:CF_SIDX + sc * 16 + 1], [[1, 16], [0, 64]]), op=ALU.is_le),
           [f"cum{cur}", "cf"], ["scr"])
        dv(lambda e: e.tensor_reduce(out=EF[:, sc * 16:(sc + 1) * 16], in_=scr, axis=AX.X, op=ALU.add), ["scr"], ["EF2"])
    dv(lambda e: e.tensor_scalar(out=EF[:, :], in0=EF[:, :], scalar1=128.0, scalar2=cf[:, CF_PIDX:CF_PIDX + 1], op0=ALU.mult, op1=ALU.add),
       ["EF2", "cf", "EF"], ["EF2"])
    dv(lambda e: e.tensor_copy(out=WIDX[:, :], in_=EF[:, :]), ["EF2"], ["WIDX"])
    S.dma("sp", g2b[:, :], gf_d.partition_broadcast(128), writes=["g2b"])
    S.barrier()
    full = (stop_after is None)
    sbuf4 = [d_f32[2 + j].bitcast(BF16).rearrange("p (i n) -> p i n", i=2) for j in range(4)]
    cnt_ = 0
    for s in (range(nseq) if full else []):
        for tp in range(NT // 2):
            j = cnt_ % 4
            cnt_ += 1
            r0 = s * S_LEN + tp * 256
            S.dma("sp", sbuf4[j], h2s_d[r0:r0 + 256, :].rearrange("(i p) d -> p i d", p=128), writes=[f"hst{j}"])
            for ii in range(2):
                t = tp * 2 + ii
                for ch in range(2):
                    S.idma(hs_d[:, :], IndirectOffsetOnAxis(ap=DESTI[:, s, ch * 16 + t:ch * 16 + t + 1], axis=0), sbuf4[j][:, ii, :], None,
                           reads=[f"hst{j}", "DESTI"])
    S.barrier()
    wg_rows, wu_rows, wd_rows = wg_d[:, :], wu_d[:, :], wd_d[:, :]

    def load_weights(slot):
        kw_ = slot % 3
        off = IndirectOffsetOnAxis(ap=WIDX[:, slot:slot + 1], axis=0)
        for (wt, rows, key) in [(wgs[kw_], wg_rows, f"wg{kw_}"), (wus[kw_], wu_rows, f"wu{kw_}"), (wds[kw_], wd_rows, f"wd{kw_}")]:
            S.idma(wt.rearrange("p c n -> p (c n)"), None, rows, off, reads=["WIDX"], writes=[key], bounds_check=bc_reg, oob_is_err=False)

    rowb = [d_bf[0], d_bf[1], R1[:, 65536:67584].bitcast(BF16)]

    def load_rows(hsl):
        k3 = hsl % 3
        S.dma("sp", rowb[k3], hs_d[hsl * 128:(hsl + 1) * 128, :], writes=[f"hsb{k3}"])

    nh = NHALF if full else 0
    if full:
        load_weights(0)
        load_weights(1)
        load_rows(0)
        load_rows(1)
    for hsl in range(nh):
        k = hsl % 2
        slot = hsl // 2
        kw_ = slot % 3
        if hsl + 1 < nh:
            if hsl % 2 == 0 and slot + 2 < NSLOT:
                load_weights(slot + 2)
            if hsl + 2 < nh:
                load_rows(hsl + 2)
        hT = d_bf[2 + k].rearrange("p (c n) -> p c n", c=8)
        transposes(rowb[hsl % 3], 6 + k, 8, 128, [f"hsb{hsl % 3}"])
        S.op("dve", lambda e: e.tensor_copy(out=hT, in_=psb(6 + k).rearrange("p (c n) -> p c n", c=8)), reads=[PSK(6 + k)], writes=[f"hT{k}"])
        gub = k
        for gi, (wt, wk) in enumerate([(wgs[kw_], f"wg{kw_}"), (wus[kw_], f"wu{kw_}")]):
            for f in range(2):
                col = gi * 256 + f * 128
                for c in range(8):
                    S.op("pe", lambda e: e.matmul(ps[:, gub, col:col + 128], lhsT=wt[:, c, f * 128:(f + 1) * 128], rhs=hT[:, c, :], start=(c == 0), stop=(c == 7)),
                         reads=[wk, f"hT{k}"], writes=[PSK(gub)], inc=(c == 7))
        sgs = d_f32[8 + k][:, 0:256]
        heT = d_bf[4 + k][:, 0:256]
        S.op("act", lambda e: e.activation(out=sgs, in_=ps[:, gub, 0:256], func=AF.Silu), reads=[PSK(gub)], writes=[f"sgs{k}"])
        S.op("dve", lambda e: e.tensor_tensor(out=heT, in0=sgs, in1=ps[:, gub, 256:512], op=ALU.mult), reads=[f"sgs{k}", PSK(gub)], writes=[f"heT{k}"])
        yb = 2 + 2 * k
        for half in range(2):
            for f in range(2):
                S.op("pe", lambda e: e.matmul(ps[:, yb + half, :], lhsT=heT[:, f * 128:(f + 1) * 128], rhs=wds[kw_][:, f, half * 512:(half + 1) * 512],
                                              start=(f == 0), stop=(f == 1)),
                     reads=[f"heT{k}", f"wd{kw_}"], writes=[PSK(yb + half)], inc=(f == 1))
        ysb = d_f32[k]
        S.op("act", lambda e: e.copy(out=ysb[:, 0:512], in_=ps[:, yb, :]), reads=[PSK(yb)], writes=[f"ysb{k}"])
        S.op("dve", lambda e: e.tensor_copy(out=ysb[:, 512:1024], in_=ps[:, yb + 1, :]), reads=[PSK(yb + 1)], writes=[f"ysb{k}"])
        S.dma("act", ys_d[hsl * 128:(hsl + 1) * 128, :], ysb, reads=[f"ysb{k}"])
    S.barrier()
    ntile_all = nseq * NT if full else 0

    def cmb_load(idx):
        s_, i_ = divmod(idx, NT)
        j = idx % 3
        S.dma("sp", d_f32[6 + j], xmid_d[s_ * S_LEN + i_ * 128:s_ * S_LEN + (i_ + 1) * 128, :], writes=[f"xmt{j}"])
        S.idma(d_f32[j], None, ys_d[:, :], IndirectOffsetOnAxis(ap=DESTI[:, s_, i_:i_ + 1], axis=0), reads=["DESTI"], writes=[f"y1{j}"])
        S.idma(d_f32[3 + j], None, ys_d[:, :], IndirectOffsetOnAxis(ap=DESTI[:, s_, 16 + i_:17 + i_], axis=0), reads=["DESTI"], writes=[f"y2{j}"])

    for idx in range(min(2, ntile_all)):
        cmb_load(idx)
    for idx in range(ntile_all):
        s_, i = divmod(idx, NT)
        j = idx % 3
        k = idx % 2
        if idx + 2 < ntile_all:
            cmb_load(idx + 2)
        y1, y2, xmt = d_f32[j], d_f32[3 + j], d_f32[6 + j]
        S.op("dve", lambda e: e.scalar_tensor_tensor(out=xmt, in0=y1, scalar=WV[:, s_, i:i + 1], in1=xmt, op0=ALU.mult, op1=ALU.add),
             reads=[f"y1{j}", f"xmt{j}", "WV"], writes=[f"xmt{j}"])
        S.op("dve", lambda e: e.scalar_tensor_tensor(out=xmt, in0=y2, scalar=WV[:, s_, 16 + i:17 + i], in1=xmt, op0=ALU.mult, op1=ALU.add),
             reads=[f"y2{j}", f"xmt{j}", "WV"], writes=[f"xmt{j}"])
        S.op("act", lambda e: e.activation(out=d_bf[k], in_=xmt, func=AF.Square, accum_out=st[k][:, 4:5]),
             reads=[f"xmt{j}"], writes=[f"dbf{k}", f"ssf{k}"])
        rstd(st[k][:, 4:5], st[k][:, 5:6], f"ssf{k}", f"rstdf{k}")
        S.op("dve", lambda e: e.scalar_tensor_tensor(out=y1, in0=xmt, scalar=st[k][:, 5:6], in1=g2b[:, :], op0=ALU.mult, op1=ALU.mult),
             reads=[f"xmt{j}", f"rstdf{k}", "g2b"], writes=[f"y1{j}"])
        S.dma("act", out_d[s_, i * 128:(i + 1) * 128, :], y1, reads=[f"y1{j}"])
    S.barrier()
    if debug:
        for name, (ap, shape, dty) in debug_taps(locals()).items():
            d = dbg_out(name, shape, dty)
            S.dma("sp", d, ap, reads=[])
        S.barrier()
    print(f"[build] instructions={S.n_ins} waits={S.n_wait} counts={S.cnt}")
    return nc, dbg


def debug_taps(L):
    return {
        "KT": (L["KT"][:, :, :], [128, 8, S_LEN], BF16),
        "Vd": (L["Vd"][:, :, :, :], [128, NT, 8, 65], BF16),
        "VS": (L["VS"][:, :, :, :], [128, NT, 4, 65], BF16),
        "KCT": (L["KCT"][:, :], [128, 128], BF16),
        "VCX": (L["VCX"][:, :, :], [128, 2, 97], BF16),
        "xT": (L["xT"][:, :, :], [128, 8, S_LEN], BF16),
        "mix": (L["mix"][:, :], [128, D], BF16),
        "OH": (L["OH"][:, :, :, :], [128, 2, NT, NE], BF16),
        "WV": (L["WV"][:, :, :], [128, 4, 32], F32),
        "DESTI": (L["DESTI"][:, :, :], [128, 4, 32], I32),
        "WIDX": (L["WIDX"][:, :], [128, NSLOT], I32),
        "gate": (L["gate"][:, :], [128, 24], F32),
    }


_CACHE = {}


def prep_weights(norm1_g, w_in, pe_kc, w_kc1, w_kc2, pe_vc, w_vc1, w_vc2, w_o, norm2_g, w_rg, b_rg, w_re, b_re,
                 w_gate, w_up, w_down, norm_f_g):
    f = lambda a: np.ascontiguousarray(np.asarray(a, dtype=np.float32))
    kvp, qp = w_in_perm()
    w_in0 = f(w_in)[0]
    pm = lambda w: np.ascontiguousarray(w.reshape(8, 128, -1).transpose(1, 0, 2))
    cbv, cfv = host_constants()
    peT = np.zeros((128, 2, 32), np.float32)
    for kv, pe in enumerate((f(pe_kc)[0], f(pe_vc)[0])):
        peT[0:64, kv, :] = pe.T
        peT[64:128, kv, :] = pe.T
    w1 = np.stack([f(w_kc1)[0], f(w_vc1)[0]])
    w1 = w1.reshape(2, 32, 64, 256).transpose(0, 2, 1, 3)
    w1 = np.ascontiguousarray(np.concatenate([w1, w1], axis=1))
    w2 = np.stack([f(w_kc2)[0], f(w_vc2)[0]])
    w2 = np.ascontiguousarray(w2.reshape(2, 2, 128, 64).transpose(0, 2, 1, 3))
    w_r = np.concatenate([f(w_rg)[0], f(w_re)[0].reshape(D, 64)], axis=1)
    b_r = np.concatenate([f(b_rg)[0], f(b_re)[0].reshape(64)])
    wg = np.ascontiguousarray(f(w_gate)[0].reshape(NE, 8, 128, DFF).transpose(0, 2, 1, 3)).reshape(NE * 128, 8 * DFF)
    wu = np.ascontiguousarray(f(w_up)[0].reshape(NE, 8, 128, DFF).transpose(0, 2, 1, 3)).reshape(NE * 128, 8 * DFF)
    wd = np.ascontiguousarray(f(w_down)[0].reshape(NE, 2, 128, D).transpose(0, 2, 1, 3)).reshape(NE * 128, 2 * D)
    return {
        "w_kv": pm(w_in0[:, kvp]), "w_q": pm(w_in0[:, qp]), "w_o": pm(f(w_o)[0]), "w_r": pm(w_r), "b_r": b_r,
        "g1": f(norm1_g)[0], "g2": f(norm2_g)[0], "gf": f(norm_f_g), "peT": peT, "w_c1": w1, "w_c2": w2,
        "w_gate": wg, "w_up": wu, "w_down": wd, "cb": cbv, "cf": cfv,
    }


def kernel(x, norm1_g, w_in, pe_kc, w_kc1, w_kc2, pe_vc, w_vc1, w_vc2, w_o, norm2_g, w_rg, b_rg, w_re, b_re,
           w_gate, w_up, w_down, norm_f_g):
    x = np.asarray(x, dtype=np.float32)
    nseq = B_TOTAL // N_CORES
    W = prep_weights(norm1_g, w_in, pe_kc, w_kc1, w_kc2, pe_vc, w_vc1, w_vc2, w_o, norm2_g, w_rg, b_rg, w_re, b_re,
                     w_gate, w_up, w_down, norm_f_g)
    nc, _ = build(nseq)
    in_maps = []
    for c in range(N_CORES):
        m = dict(W)
        m["x"] = np.ascontiguousarray(x[c * nseq:(c + 1) * nseq])
        in_maps.append(m)
    res = run_bass_kernel_spmd(nc, in_maps, core_ids=list(range(N_CORES)))
    return np.concatenate([np.asarray(r["out"], dtype=np.float32) for r in res.results], axis=0)
```

```python
import contextlib
import numpy as np
import ml_dtypes
import concourse.bass as bass
import concourse.mybir as mybir
from concourse.ap import AP
from concourse.bass_utils import run_bass_kernel_spmd

F32 = mybir.dt.float32
BF16 = mybir.dt.bfloat16
U8 = mybir.dt.uint8
I32 = mybir.dt.int32
from concourse.bass import IndirectOffsetOnAxis
ALU = mybir.AluOpType
AF = mybir.ActivationFunctionType
AX = mybir.AxisListType

D = 1024
S_LEN = 2048
NT = 16
HD = 64
N_CORES = 8
B_TOTAL = 32
D_IN = 2840
NKV = 1792
NQ = 1048
NE = 64
DFF = 256
EPS = 1e-6
NEGM = -30000.0
import os
CUT = int(os.environ.get("KCUT", "9"))
STRICT = os.environ.get("KSTRICT", "1") == "1"
PAIR = os.environ.get("KPAIR", "0") == "1"

CB_ID = 0
CB_MALL = 128
CB_CM = CB_MALL + 2048
CB_TRI = CB_CM + 2048
CB_TRIW = CB_TRI + 128
CB_E = CB_TRIW + 128
CB_UT = CB_E + 2048
CB_ONES = CB_UT + 128
CB_N = CB_ONES + 128
CF_SA = 0
CF_SB = 512
CF_COS = 1024
CF_SIN = 1152
CF_COSC = 1280
CF_SINC = 1288
CF_OV = 1296
CF_THR = 1328
CF_SIDX = 1360
CF_PIDX = 1488
CF_N = 1492
NSLOT = 128
SROWS = 256
NHALF = NSLOT * 2


class Sync:
    NDMA = 40

    def __init__(self, nc, es):
        self.nc = nc
        self.engs = {"pe": nc.tensor, "dve": nc.vector, "act": nc.scalar, "pool": nc.gpsimd, "sp": nc.sync}
        self.sem = {k: es.enter_context(nc.semaphore(f"sem_{k}")) for k in ("pe", "dve", "act", "pool")}
        self.cnt = {k: 0 for k in self.sem}
        self.pending = {k: False for k in self.sem}
        self.waited = {k: {} for k in self.engs}
        self.dsem = [es.enter_context(nc.semaphore(f"dsem{i}")) for i in range(self.NDMA)]
        self.dcnt = [0] * self.NDMA
        self.dnext = {"sp": 0, "pool": 0, "act": 0}
        self.drange = {"sp": (0, 24), "pool": (24, 40), "act": (0, 24)}
        self.lastw = {}
        self.readers = {}
        self.all_dma_tokens = {}
        self.n_ins = 0
        self.n_wait = 0

    def _semobj(self, k):
        return self.sem[k] if isinstance(k, str) else self.dsem[k]

    def _wait(self, eng, tok, raw):
        if tok is None:
            return
        k, v = tok
        if k == eng:
            if eng == "pe" or (not raw and not STRICT):
                return
        if self.waited[eng].get(k, 0) >= v:
            return
        self.engs[eng].wait_ge(self._semobj(k), v)
        self.waited[eng][k] = v
        self.n_wait += 1

    def _deps(self, eng, reads, writes):
        for b in reads:
            self._wait(eng, self.lastw.get(b), True)
            if isinstance(b, tuple) and b[0] == "ps":
                for tk in self.readers.get(b, ()):
                    self._wait(eng, tk, False)
        for b in writes:
            self._wait(eng, self.lastw.get(b), False)
            for tk in self.readers.get(b, ()):
                self._wait(eng, tk, False)

    def _record(self, tok, reads, writes):
        for b in reads:
            lst = self.readers.setdefault(b, [])
            lst[:] = [t for t in lst if t[0] != tok[0]]
            lst.append(tok)
        for b in writes:
            self.lastw[b] = tok
            self.readers[b] = []

    def op(self, eng, fn, reads=(), writes=(), inc=True):
        self._deps(eng, reads, writes)
        ins = fn(self.engs[eng])
        self.n_ins += 1
        if inc:
            self.cnt[eng] += 1
            ins.then_inc(self.sem[eng], 1)
            tok = (eng, self.cnt[eng])
            self.pending[eng] = False
        else:
            tok = (eng, self.cnt[eng] + 1)
            self.pending[eng] = True
        self._record(tok, reads, writes)
        return tok

    def dma(self, q, out, in_, reads=(), writes=(), **kw):
        lo_, hi_ = self.drange[q]
        j = lo_ + self.dnext[q]
        self.dnext[q] = (self.dnext[q] + 1) % (hi_ - lo_)
        if self.dcnt[j] > 0:
            self._wait(q, (j, 16 * self.dcnt[j]), False)
        self._deps(q, reads, writes)
        if q == "pool":
            kw.setdefault("max_dma_last_dim", 2048)
        ins = self.engs[q].dma_start(out=out, in_=in_, **kw)
        self.dcnt[j] += 1
        ins.then_inc(self.dsem[j], 16)
        tok = (j, 16 * self.dcnt[j])
        self.all_dma_tokens[j] = tok
        self.n_ins += 1
        self._record(tok, reads, writes)
        return tok

    def idma(self, out, out_off, in_, in_off, reads=(), writes=(), **kw):
        q = "pool"
        lo_, hi_ = self.drange[q]
        j = lo_ + self.dnext[q]
        self.dnext[q] = (self.dnext[q] + 1) % (hi_ - lo_)
        if self.dcnt[j] > 0:
            self._wait(q, (j, 16 * self.dcnt[j]), False)
        self._deps(q, reads, writes)
        ins = self.engs[q].indirect_dma_start(out=out, out_offset=out_off, in_=in_, in_offset=in_off, **kw)
        self.dcnt[j] += 1
        ins.then_inc(self.dsem[j], 16)
        tok = (j, 16 * self.dcnt[j])
        self.all_dma_tokens[j] = tok
        self.n_ins += 1
        self._record(tok, reads, writes)
        return tok

    def barrier(self):
        for k in self.sem:
            assert not self.pending[k], f"pending non-inc instruction on {k}"
        for e in self.engs:
            for k in self.sem:
                if self.cnt[k] > 0:
                    self._wait(e, (k, self.cnt[k]), True)
            for j, tok in self.all_dma_tokens.items():
                self._wait(e, tok, True)
        self.lastw.clear()
        self.readers.clear()


def mk_ap(base, dims):
    return AP(base.tensor, base.offset, [list(base.ap[0])] + [list(d) for d in dims])


def host_constants():
    cb = np.zeros((128, CB_N), np.float32)
    cb[:, CB_ID:CB_ID + 128] = np.eye(128)
    ki = np.arange(128)[:, None]
    qi = np.arange(128)[None, :]
    for m in range(16):
        dist = (15 - m) * 128 + qi - ki
        c = ((dist >= 0) & (dist <= 128)).astype(np.float32)
        c += ((dist >= 0) & (dist % 4 == 0) & (dist <= 512))
        c += ((dist >= 0) & (dist % 16 == 0) & (dist <= 2048))
        cb[:, CB_MALL + m * 128:CB_MALL + (m + 1) * 128] = c
    cidx = np.arange(128)[:, None]
    for t in range(16):
        q = t * 128 + qi
        cb[:, CB_CM + t * 128:CB_CM + (t + 1) * 128] = ((16 * cidx + 31 <= q) & (cidx < 127))
    cb[:, CB_TRI:CB_TRI + 128] = (ki <= qi)
    cb[:, CB_TRIW:CB_TRIW + 128] = (qi < ki)
    kk = np.arange(2048)[None, :]
    jj = np.arange(128)[:, None]
    cb[:, CB_E:CB_E + 2048] = ((kk // 64 == (jj % 64)) & ((jj % 64) < 32))
    cb[:, CB_UT:CB_UT + 128] = (ki < qi)
    cb[:, CB_ONES:CB_ONES + 128] = 1.0
    cf = np.zeros((128, CF_N), np.float32)
    cf[:, CF_THR:CF_THR + 32] = float(SROWS) * np.arange(32)[None, :]
    cf[:, CF_SIDX:CF_SIDX + NSLOT] = np.arange(NSLOT)[None, :]
    cf[:, CF_PIDX] = np.arange(128)
    p = np.arange(128)[:, None]
    j = np.arange(32)[None, :]
    for t in range(16):
        tq = t * 128 + p
        blk = tq // 64
        forced = (j == 0) | (j == blk) | (j == blk - 1)
        valid = (j * 64 <= tq)
        cf[:, CF_SA + t * 32:CF_SA + (t + 1) * 32] = (valid & ~forced)
        cf[:, CF_SB + t * 32:CF_SB + (t + 1) * 32] = np.where(valid, np.where(forced, 1e9, 0.0), -1e9)
    inv_freq = (500000.0 ** (-np.arange(0, 16, 2, dtype=np.float32) / 16)).astype(np.float32)
    pos = np.arange(2048, dtype=np.float32)
    ang = pos[:, None] * inv_freq[None, :]
    cos = np.cos(ang).astype(np.float32).reshape(16, 128, 8).transpose(1, 0, 2).reshape(128, 128)
    sin = np.sin(ang).astype(np.float32).reshape(16, 128, 8).transpose(1, 0, 2).reshape(128, 128)
    cf[:, CF_COS:CF_COS + 128] = cos
    cf[:, CF_SIN:CF_SIN + 128] = sin
    endp = (np.arange(127) * 16 + 31).astype(np.float32)
    angc = endp[:, None] * inv_freq[None, :]
    cf[:127, CF_COSC:CF_COSC + 8] = np.cos(angc)
    cf[:127, CF_SINC:CF_SINC + 8] = np.sin(angc)
    cs = np.arange(127)[:, None] * 16
    js = np.arange(32)[None, :] * 64
    ov = np.clip(np.minimum(cs + 32, js + 64) - np.maximum(cs, js), 0, None).astype(np.float32) / 32
    cf[:127, CF_OV:CF_OV + 32] = ov
    return cb.astype(ml_dtypes.bfloat16), cf


def w_in_perm():
    aq, ak, av, bq, kc, vc, ks, vs, kw, vw, gt = 0, 512, 1024, 1536, 2048, 2176, 2304, 2432, 2560, 2688, 2816
    r = lambda a, n: list(range(a, a + n))
    kv = r(ak, 512) + r(ks, 128) + r(kw, 128) + r(kc, 128) + r(vc, 128) + r(av, 512) + r(vs, 128) + r(vw, 128)
    q = r(aq, 512)
    for rr in range(4):
        for g in range(2):
            q += r(bq + (g * 4 + rr) * 64, 64)
    q += r(gt, 24)
    assert len(kv) == NKV and len(q) == NQ
    return np.array(kv), np.array(q)


def build(nseq, debug=False, stop_after=None):
    nc = bass.Bass("TRN2", target_bir_lowering=False)
    es = contextlib.ExitStack()
    dt = lambda name, shape, dty=F32, kind="ExternalInput": nc.dram_tensor(name, shape, dty, kind=kind).ap()
    x_d = dt("x", [nseq, S_LEN, D])
    out_d = dt("out", [nseq, S_LEN, D], kind="ExternalOutput")
    wkv_d = dt("w_kv", [128, 8, NKV])
    wq_d = dt("w_q", [128, 8, NQ])
    wo_d = dt("w_o", [128, 8, D])
    wr_d = dt("w_r", [128, 8, 72])
    br_d = dt("b_r", [72])
    g1_d = dt("g1", [D])
    g2_d = dt("g2", [D])
    gf_d = dt("gf", [D])
    peT_d = dt("peT", [128, 2, 32])
    w1_d = dt("w_c1", [2, 128, 32, 256])
    w2_d = dt("w_c2", [2, 128, 2, 64])
    NEd = NE if stop_after is None else 1
    wg_d = dt("w_gate", [NEd * 128, 8 * DFF])
    wu_d = dt("w_up", [NEd * 128, 8 * DFF])
    wd_d = dt("w_down", [NEd * 128, 2 * D])
    cb_d = dt("cb", [128, CB_N], BF16)
    cf_d = dt("cf", [128, CF_N])
    xmid_d = nc.dram_tensor("xmid", [nseq * S_LEN, D], F32, kind="Internal").ap()
    h2s_d = nc.dram_tensor("h2s", [nseq * S_LEN, D], BF16, kind="Internal").ap()
    oh_d = nc.dram_tensor("ohd", [nseq, 128, 2 * NT * NE], BF16, kind="Internal").ap()
    hs_d = nc.dram_tensor("hs", [NSLOT * SROWS, D], BF16, kind="Internal").ap()
    ys_d = nc.dram_tensor("ys", [NSLOT * SROWS, D], F32, kind="Internal").ap()
    dbg = {}

    def dbg_out(name, shape, dty=F32):
        dbg[name] = dt("dbg_" + name, shape, dty, kind="ExternalOutput")
        return dbg[name]

    sb = lambda name, shape, dty: es.enter_context(nc.sbuf_tensor("s_" + name, shape, dty))
    cb = sb("cb", [128, CB_N], BF16)
    cf = sb("cf", [128, CF_N], F32)
    g2b = sb("gb", [128, D], F32)
    g1b = g2b
    brb = sb("brb", [128, 72], F32)
    wr = sb("wr", [128, 8, 72], BF16)
    peT = sb("peT", [128, 2, 32], F32)
    w2 = sb("w2", [128, 2, 2, 64], BF16)
    xT = sb("xT", [128, 8, S_LEN], BF16)
    OH = sb("OH", [128, 2, NT, NE], BF16)
    RK = sb("RK", [128, 4, 32], F32)
    WV = sb("WV", [128, 4, 32], F32)
    run = sb("run", [128, NE], F32)
    DESTI = sb("DESTI", [128, 4, 32], I32)
    WIDX = sb("WIDX", [128, NSLOT], I32)
    cum = [sb(f"cum{i}", [128, NE], F32) for i in range(2)]
    ntl = sb("ntl", [128, NE], F32)
    EF = sb("EF", [128, NSLOT], F32)
    KCT = sb("KCT", [128, 128], BF16)
    VCX = sb("VCX", [128, 2, 97], BF16)
    R1_BYTES = 32768 + 16640 + 8320 + NQ * 16
    R1 = sb("R1", [128, R1_BYTES], U8)
    o = 0

    def carve(nbytes):
        nonlocal o
        v = R1[:, o:o + nbytes]
        o += nbytes
        return v

    KT = carve(32768).bitcast(BF16).rearrange("p (b n) -> p b n", b=8)
    Vd = carve(16640).bitcast(BF16).rearrange("p (i h e) -> p i h e", i=NT, h=8)
    VS = carve(8320).bitcast(BF16).rearrange("p (i h e) -> p i h e", i=NT, h=4)
    wq = carve(NQ * 16).bitcast(BF16).rearrange("p (c n) -> p c n", c=8)
    R2_BYTES = NKV * 16
    R2 = sb("R2", [128, R2_BYTES], U8)
    wkv = R2[:, 0:NKV * 16].bitcast(BF16).rearrange("p (c n) -> p c n", c=8)
    w1 = R2[:, 0:16384].bitcast(BF16).rearrange("p (q h) -> p q h", q=32)
    Z = R2[:, 16384:16384 + 8192].bitcast(BF16).rearrange("p (q c) -> p q c", q=32)
    wo = R2[:, 0:16384].bitcast(BF16).rearrange("p (c n) -> p c n", c=8)
    d_f32 = [R1[:, i * 4096:(i + 1) * 4096].bitcast(F32) for i in range(10)]
    d_bf = [R1[:, 40960 + i * 2048:40960 + (i + 1) * 2048].bitcast(BF16) for i in range(6)]
    o2 = 0
    wgs, wus, wds = [], [], []
    for sl in range(2):
        wgs.append(R2[:, o2:o2 + 4096].bitcast(BF16).rearrange("p (c n) -> p c n", c=8)); o2 += 4096
        wus.append(R2[:, o2:o2 + 4096].bitcast(BF16).rearrange("p (c n) -> p c n", c=8)); o2 += 4096
        wds.append(R2[:, o2:o2 + 4096].bitcast(BF16).rearrange("p (c n) -> p c n", c=2)); o2 += 4096
    o3 = 53248
    wgs.append(R1[:, o3:o3 + 4096].bitcast(BF16).rearrange("p (c n) -> p c n", c=8)); o3 += 4096
    wus.append(R1[:, o3:o3 + 4096].bitcast(BF16).rearrange("p (c n) -> p c n", c=8)); o3 += 4096
    wds.append(R1[:, o3:o3 + 4096].bitcast(BF16).rearrange("p (c n) -> p c n", c=2)); o3 += 4096
    assert o <= R1_BYTES and o2 <= R2_BYTES and 40960 + 6 * 2048 <= 53248 and o3 <= R1_BYTES
    xt = [sb(f"xt{i}", [128, D], F32) for i in range(2)]
    xn = [sb(f"xn{i}", [128, D], BF16) for i in range(2)]
    ktm = [sb(f"ktm{i}", [128, 1024], BF16) for i in range(2)]
    st = [sb(f"st{i}", [128, 8], F32) for i in range(2)]
    rt = [sb(f"rt{i}", [128, 4, 128], F32) for i in range(2)]
    QZd = sb("QZd", [128, 8, 128], BF16)
    QZn = sb("QZn", [128, 2, 4, 128], BF16)
    Pp = [sb(f"Pp{i}", [128, 1024], BF16) for i in range(2)]
    Pb = [Pp[0][:, 0:512], Pp[0][:, 512:1024], Pp[1][:, 0:512], Pp[1][:, 512:1024]]
    mix = xn[1]
    mixT = sb("mixT", [128, 8, 128], BF16)
    gate = sb("gate", [128, 24], F32)
    sm = sb("sm", [128, 64], F32)
    imp = sb("imp", [128, 4, 32], F32)
    selb = sb("selb", [128, 96], BF16)
    xm = sb("xm", [128, D], F32)
    h2 = xn[0]
    rs = sb("rs", [128, 256], F32)
    he = [xn[0][:, 0:512], xn[0][:, 512:1024], xn[1][:, 0:512], xn[1][:, 512:1024]]
    sg = [xm[:, 0:512], xm[:, 512:1024]]
    ot = xt
    ps = es.enter_context(nc.psum_tensor("ps", [128, 8, 512], F32))
    psb = lambda b: ps[:, b, :].bitcast(BF16)

    S = Sync(nc, es)
    ident = cb[:, CB_ID:CB_ID + 128]
    PSK = lambda b: ("ps", b)

    S.dma("sp", cb[:, :], cb_d[:, :], writes=["cb"])
    S.dma("sp", cf[:, :], cf_d[:, :], writes=["cf"])
    S.dma("sp", brb[:, :], br_d.partition_broadcast(128), writes=["brb"])
    S.dma("sp", peT[:, :, :], peT_d[:, :, :], writes=["peT"])
    S.dma("pool", wr[:, :, :], wr_d[:, :, :], writes=["wr"])
    for kv in range(2):
        S.dma("pool", w2[:, kv, :, :], w2_d[kv], writes=["w2"])
    S.barrier()

    eps_t = sb("eps_t", [128, 1], F32)
    tmpb = sb("tmpb", [128, 4, 64], F32)
    maddT = sb("maddT", [128, 2, 4, 128], BF16)
    accg = sb("accg", [128, 2, 4, 64], F32)
    S.op("dve", lambda e: e.memset(eps_t[:, :], EPS), writes=["eps"])
    S.op("dve", lambda e: e.memset(selb[:, :], 0.0), writes=["selb"])
    S.dma("pool", wq[:, :, :], wq_d[:, :, :], writes=["wq"])
    S.op("pool", lambda e: e.memset(Vd[:, :, :, 64:65], 1.0), writes=["Vd1"])
    S.op("pool", lambda e: e.memset(VS[:, :, :, 64:65], 1.0), writes=["VS1"])
    S.op("dve", lambda e: e.memset(run[:, :], 0.0), writes=["run"])
    S.op("dve", lambda e: e.memset(QZd[:, :, :], 0.0), writes=["QZ"])
    S.op("dve", lambda e: e.memset(QZn[:, :, :, :], 0.0), writes=["QZ"])
    S.op("dve", lambda e: e.memset(maddT[:, :, :, :], 0.0), writes=["madd0", "madd1"])
    zt = sb("zt", [128, 256], BF16)
    S.op("dve", lambda e: e.memset(zt[:, :], 0.0), writes=["zt"])
    zf_state = {"next": 0, "total": (NSLOT * SROWS // 128) if stop_after is None else 0}

    def zero_fill_one():
        for _ in range(4):
            zi = zf_state["next"]
            if zi >= zf_state["total"]:
                return
            zf_state["next"] = zi + 1
            S.dma("sp", hs_d[zi * 128:(zi + 1) * 128, :].rearrange("p (j d) -> p j d", d=256),
                  mk_ap(zt[:, 0:1], [[0, 4], [1, 256]]), reads=["zt"])
    S.op("dve", lambda e: e.memset(VCX[:, :, 64:65], 1.0), writes=["VCX1"])
    for g in range(2):
        S.op("dve", lambda e: e.tensor_copy(out=VCX[0:127, g, 65:97], in_=cf[0:127, CF_OV:CF_OV + 32]), reads=["cf"], writes=["VCX2"])
    S.barrier()
    psflat = ps[:, :, :].rearrange("p b n -> p (b n)")

    def rstd(ss_ap, out_ap, kin, kout):
        S.op("act", lambda e: e.activation(out=out_ap, in_=ss_ap, func=AF.Ln, scale=1.0 / D, bias=eps_t[:, 0:1]),
             reads=[kin, "eps"], writes=[kout])
        S.op("act", lambda e: e.activation(out=out_ap, in_=out_ap, func=AF.Exp, scale=-0.5), reads=[kout], writes=[kout])

    def rope(src3, dst3, nh, cs, sn, npart, ri, src_keys, dst_keys):
        r = rt[ri]
        x1 = src3[:, :, 0:8]
        x2 = src3[:, :, 8:16]
        tmp = lambda j: r[0:npart, j, 0:nh * 8].rearrange("p (h e) -> p h e", h=nh)
        rk = f"rt{ri}"
        for j, (xx, tb) in enumerate([(x1, cs), (x2, sn), (x2, cs), (x1, sn)]):
            S.op("dve", lambda e: e.tensor_tensor(out=tmp(j), in0=xx, in1=tb, op=ALU.mult), reads=src_keys + ["cf"], writes=[rk])
        S.op("dve", lambda e: e.tensor_tensor(out=dst3[:, :, 0:8], in0=tmp(0), in1=tmp(1), op=ALU.subtract), reads=[rk], writes=dst_keys)
        S.op("dve", lambda e: e.tensor_tensor(out=dst3[:, :, 8:16], in0=tmp(2), in1=tmp(3), op=ALU.add), reads=[rk], writes=dst_keys)
        S.op("act", lambda e: e.copy(out=dst3[:, :, 16:64], in_=src3[:, :, 16:64]), reads=src_keys, writes=dst_keys)

    def tab(off, i, nh, npart=128):
        return mk_ap(cf[0:npart, off + i * 8:off + i * 8 + 1], [[0, nh], [1, 8]])

    def transposes(src2d, bank, nblk, npart, src_keys):
        for c in range(nblk):
            S.op("pe", lambda e: e.transpose(out=psb(bank)[:, c * 128:c * 128 + npart], in_=src2d[0:npart, c * 128:(c + 1) * 128],
                                             identity=cb[0:npart, CB_ID:CB_ID + npart]),
                 reads=src_keys + ["cb"], writes=[PSK(bank)], inc=(c == nblk - 1))

    bc_reg = nc.gpsimd.alloc_register("bcreg")
    nc.gpsimd.reg_mov(bc_reg, NE * 128 - 1)
    for s in range(nseq):
        S.dma("pool", wkv[:, :, :], wkv_d[:, :, :], writes=["wkv"])
        S.dma("sp", g1b[:, :], g1_d.partition_broadcast(128), writes=["g1b"])

        def pa_front(i):
            k = i % 2
            tsl = slice(i * 128, (i + 1) * 128)
            S.dma("sp", xt[k][:, :], x_d[s, tsl, :], writes=[f"xt{k}"])
            S.op("act", lambda e: e.activation(out=xn[k][:, :], in_=xt[k][:, :], func=AF.Square, accum_out=st[k][:, 0:1]),
                 reads=[f"xt{k}"], writes=[f"xn{k}", f"ss{k}"])
            rstd(st[k][:, 0:1], st[k][:, 1:2], f"ss{k}", f"rstd{k}")
            S.op("dve", lambda e: e.scalar_tensor_tensor(out=xn[k][:, :], in0=xt[k][:, :], scalar=st[k][:, 1:2], in1=g1b[:, :],
                                                         op0=ALU.mult, op1=ALU.mult),
                 reads=[f"xt{k}", f"rstd{k}", "g1b"], writes=[f"xn{k}"])
            transposes(xn[k], 7, 8, 128, [f"xn{k}"])
            S.op("dve", lambda e: e.tensor_copy(out=xT[:, :, tsl], in_=psb(7).rearrange("p (c n) -> p c n", c=8)),
                 reads=[PSK(7)], writes=[f"xT{i}"])

        def pa_proj(i):
            tsl = slice(i * 128, (i + 1) * 128)
            for nb, (n0, n1) in enumerate([(0, 512), (512, 1024), (1024, 1536), (1536, 1792)]):
                for c in range(8):
                    S.op("pe", lambda e: e.matmul(ps[:, nb, 0:n1 - n0], lhsT=xT[:, c, tsl], rhs=wkv[:, c, n0:n1], start=(c == 0), stop=(c == 7)),
                         reads=[f"xT{i}", "wkv"], writes=[PSK(nb)], inc=(c == 7))

        def pa_evac(i):
            k = i % 2
            src3 = psflat[:, 0:768].rearrange("p (h e) -> p h e", h=12)
            dst3 = ktm[k][:, 0:768].rearrange("p (h e) -> p h e", h=12)
            rope(src3, dst3, 12, tab(CF_COS, i, 12), tab(CF_SIN, i, 12), 128, k, [PSK(0), PSK(1)], [f"ktm{k}"])
            S.op("act", lambda e: e.copy(out=ktm[k][:, 768:1024], in_=psflat[:, 768:1024]), reads=[PSK(1)], writes=[f"ktm{k}"])
            S.op("act", lambda e: e.copy(out=Vd[:, i, :, 0:64], in_=ps[:, 2, :].rearrange("p (h e) -> p h e", h=8)),
                 reads=[PSK(2)], writes=["Vd"])
            S.op("act", lambda e: e.copy(out=VS[:, i, :, 0:64], in_=ps[:, 3, 0:256].rearrange("p (h e) -> p h e", h=4)),
                 reads=[PSK(3)], writes=["VS"])

        def pa_back(i):
            k = i % 2
            tsl = slice(i * 128, (i + 1) * 128)
            transposes(ktm[k], 6, 8, 128, [f"ktm{k}"])
            S.op("dve", lambda e: e.tensor_copy(out=KT[:, :, tsl], in_=psb(6).rearrange("p (c n) -> p c n", c=8)),
                 reads=[PSK(6)], writes=["KT"])

        pa_front(0)
        pa_proj(0)
        for i in range(NT):
            if i + 1 < NT:
                pa_front(i + 1)
            pa_evac(i)
            if i + 1 < NT:
                pa_proj(i + 1)
            pa_back(i)
        S.barrier()
        if stop_after == "A":
            break
        S.dma("sp", g2b[:, :], g2_d.partition_broadcast(128), writes=["g2b"])
        for kv in range(2):
            S.dma("pool", w1[:, :, :], w1_d[kv], writes=["w1"])
            zin = mk_ap(KT[:, 6 + kv, 0:1], [[16, 127], [1, 32]])
            pin = mk_ap(peT[:, kv, 0:1], [[0, 127], [1, 32]])
            S.op("dve", lambda e: e.tensor_tensor(out=mk_ap(Z[:, 0, 0:1], [[1, 127], [128, 32]]), in0=zin, in1=pin, op=ALU.add), reads=["KT", "peT"], writes=["Z"])
            if CUT < 2:
                continue
            KVAR = int(os.environ.get("KVAR", "0"))
            for g in ([0] if KVAR == 1 else [1] if KVAR == 4 else range(2)):
                for hc in range(2):
                    col = hc * 128
                    nq = 8 if KVAR == 2 else 32
                    for q in range(nq):
                        S.op("pe", lambda e: e.matmul(ps[:, g, col:col + 128], lhsT=w1[g * 64:(g + 1) * 64, q, hc * 128:(hc + 1) * 128],
                                                      rhs=Z[g * 64:(g + 1) * 64, q, 0:128], start=(q == 0), stop=(q == nq - 1)),
                             reads=["w1", "Z"], writes=[PSK(g)], inc=(q == nq - 1 or KVAR == 3))
            if CUT < 3:
                continue
            xs = xt[0][:, 0:512]
            uu = xt[0][:, 512:1024]
            for g in range(2):
                S.op("act", lambda e: e.copy(out=xs[:, g * 256:(g + 1) * 256], in_=ps[:, g, 0:256]), reads=[PSK(g)], writes=["xs"])
            S.op("dve", lambda e: e.tensor_tensor(out=uu, in0=xs, in1=xs, op=ALU.mult), reads=["xs"], writes=["uu"])
            S.op("dve", lambda e: e.tensor_scalar(out=uu, in0=uu, scalar1=0.044715, scalar2=1.0, op0=ALU.mult, op1=ALU.add), reads=["uu"], writes=["uu"])
            S.op("dve", lambda e: e.tensor_tensor(out=uu, in0=uu, in1=xs, op=ALU.mult), reads=["uu", "xs"], writes=["uu"])
            S.op("act", lambda e: e.activation(out=uu, in_=uu, func=AF.Tanh, scale=0.7978845608), reads=["uu"], writes=["uu"])
            S.op("dve", lambda e: e.scalar_tensor_tensor(out=uu, in0=uu, scalar=1.0, in1=xs, op0=ALU.add, op1=ALU.mult), reads=["uu", "xs"], writes=["uu"])
            S.op("act", lambda e: e.mul(out=Pb[0][:, :], in_=uu, mul=0.5), reads=["uu"], writes=["P0"])
            if CUT < 4:
                continue
            for g in range(2):
                for hc in range(2):
                    col = (g * 2 + hc) * 128
                    S.op("pe", lambda e: e.matmul(ps[0:127, 2, g * 64:(g + 1) * 64], lhsT=Pb[0][:, col:col + 127], rhs=w2[:, kv, hc, :],
                                                  start=(hc == 0), stop=(hc == 1)),
                         reads=["P0", "w2"], writes=[PSK(2)], inc=(hc == 1))
            if CUT < 5:
                continue
            src3 = ps[0:127, 2, 0:128].rearrange("p (h e) -> p h e", h=2)
            if kv == 0:
                dst3 = ktm[0][0:127, 0:128].rearrange("p (h e) -> p h e", h=2)
                rope(src3, dst3, 2, tab(CF_COSC, 0, 2, 127), tab(CF_SINC, 0, 2, 127), 127, 0, [PSK(2)], ["ktm0"])
                transposes(ktm[0], 6, 1, 127, ["ktm0"])
                S.op("dve", lambda e: e.tensor_copy(out=KCT[:, 0:127], in_=psb(6)[:, 0:127]), reads=[PSK(6)], writes=["KCT"])
            else:
                S.op("act", lambda e: e.copy(out=VCX[0:127, :, 0:64], in_=src3), reads=[PSK(2)], writes=["VCX"])
        S.barrier()
        if stop_after == "C":
            break
        S.dma("pool", wo[:, :, :], wo_d[:, :, :], writes=["wo"])
        def make_tile(t, pre_stages=()):
                qk = t % 2
                tsl = slice(t * 128, (t + 1) * 128)
                for (n0, n1, bank) in [(0, 512, 4), (512, 1024, 5), (1024, 1048, 3)]:
                    for c in range(8):
                        S.op("pe", lambda e: e.matmul(ps[:, bank, 0:n1 - n0], lhsT=xT[:, c, tsl], rhs=wq[:, c, n0:n1], start=(c == 0), stop=(c == 7)),
                             reads=[f"xT{t}", "wq"], writes=[PSK(bank)], inc=(c == 7))
                for pst in pre_stages:
                    pst()
                src3 = psflat[:, 2048:3072].rearrange("p (h e) -> p h e", h=16)
                dst3 = ktm[qk][:, :].rearrange("p (h e) -> p h e", h=16)
                rope(src3, dst3, 16, tab(CF_COS, t, 16), tab(CF_SIN, t, 16), 128, qk, [PSK(4), PSK(5)], [f"ktm{qk}"])
                S.op("act", lambda e: e.activation(out=gate[:, :], in_=ps[:, 3, 0:24], func=AF.Exp, scale=-1.0), reads=[PSK(3)], writes=["gate"])
                S.op("dve", lambda e: e.tensor_scalar_add(out=gate[:, :], in0=gate[:, :], scalar1=1.0), reads=["gate"], writes=["gate"])
                S.op("dve", lambda e: e.reciprocal(out=gate[:, :], in_=gate[:, :]), reads=["gate"], writes=["gate"])
                transposes(ktm[qk], 7, 8, 128, [f"ktm{qk}"])
                p7 = psb(7).rearrange("p (c n) -> p c n", c=8)
                QZv = QZd[:, :, :].rearrange("p (b two) n -> p b two n", two=2)
                S.op("dve", lambda e: e.tensor_copy(out=QZv[0:64, :, 0, :], in_=p7[0:64, 0:4, :]), reads=[PSK(7)], writes=["QZ"])
                S.op("dve", lambda e: e.tensor_copy(out=QZv[64:128, :, 1, :], in_=p7[64:128, 0:4, :]), reads=[PSK(7)], writes=["QZ"])
                S.op("dve", lambda e: e.tensor_copy(out=QZn[0:64, 0, :, :], in_=p7[0:64, 4:8, :]), reads=[PSK(7)], writes=["QZ"])
                S.op("dve", lambda e: e.tensor_copy(out=QZn[64:128, 1, :, :], in_=p7[64:128, 4:8, :]), reads=[PSK(7)], writes=["QZ"])
                QK_ = "QZ"
                units = []

                def gate_ap(g, b):
                    return mk_ap(gate[:, g * 12 + b:g * 12 + b + 1], [[3, 4]])

                def bc4(ap2):
                    return mk_ap(ap2, [[ap2.ap[1][0], 4], [0, 64]])

                def nsa_fin(g, bank, w, branch, first):
                    O3 = ps[:, bank, 0:4 * w].rearrange("p (r e) -> p r e", r=4)
                    S.op("dve", lambda e: e.tensor_scalar_max(out=sm[:, 4:8], in0=O3[:, :, 64], scalar1=1e-30), reads=[PSK(bank)], writes=["sm4"])
                    S.op("dve", lambda e: e.reciprocal(out=sm[:, 8:12], in_=sm[:, 4:8]), reads=["sm4"], writes=["sm8"])
                    if branch == 0:
                        S.op("dve", lambda e: e.tensor_tensor(out=imp[:, :, :], in0=O3[:, :, 65:97],
                                                              in1=mk_ap(sm[:, 8:9], [[1, 4], [0, 32]]), op=ALU.mult),
                             reads=[PSK(bank), "sm8"], writes=["imp"])
                    S.op("dve", lambda e: e.tensor_tensor(out=sm[:, 12:16], in0=sm[:, 8:12], in1=gate_ap(g, branch), op=ALU.mult),
                         reads=["sm8", "gate"], writes=["sm12"])
                    fb = mk_ap(sm[:, 12:13], [[1, 4], [0, 64]])
                    if first:
                        S.op("dve", lambda e: e.tensor_tensor(out=accg[:, g, :, :], in0=O3[:, :, 0:64], in1=fb, op=ALU.mult),
                             reads=[PSK(bank), "sm12"], writes=[f"accg{g}"])
                    else:
                        S.op("dve", lambda e: e.tensor_tensor(out=tmpb[:, :, :], in0=O3[:, :, 0:64], in1=fb, op=ALU.mult),
                             reads=[PSK(bank), "sm12"], writes=["tmpb"])
                        S.op("dve", lambda e: e.tensor_tensor(out=accg[:, g, :, :], in0=accg[:, g, :, :], in1=tmpb[:, :, :], op=ALU.add),
                             reads=["tmpb", f"accg{g}"], writes=[f"accg{g}"])

                def select(g):
                    S.op("dve", lambda e: e.tensor_reduce(out=rs[:, 0:32], in_=imp[:, :, :].rearrange("p r j -> p j r"), axis=AX.X, op=ALU.add),
                         reads=["imp"], writes=["rs0"])
                    S.op("dve", lambda e: e.tensor_tensor(out=rs[:, 0:32], in0=rs[:, 0:32], in1=cf[:, CF_SA + t * 32:CF_SA + (t + 1) * 32], op=ALU.mult),
                         reads=["rs0", "cf"], writes=["rs0"])
                    S.op("dve", lambda e: e.tensor_tensor(out=rs[:, 0:32], in0=rs[:, 0:32], in1=cf[:, CF_SB + t * 32:CF_SB + (t + 1) * 32], op=ALU.add),
                         reads=["rs0", "cf"], writes=["rs0"])
                    S.op("dve", lambda e: e.max(out=rs[:, 32:40], in_=rs[:, 0:32]), reads=["rs0"], writes=["rs32"])
                    pg_ = g * 64
                    S.op("dve", lambda e: e.tensor_scalar(out=selb[:, pg_:pg_ + 32], in0=rs[:, 0:32], scalar1=rs[:, 39:40], scalar2=NEGM, op0=ALU.is_lt, op1=ALU.mult),
                         reads=["rs0", "rs32"], writes=["selb"])

                def select_b(g):
                    pg_ = g * 64
                    S.op("pe", lambda e: e.transpose(out=psb(7)[0:96, 0:128], in_=selb[:, 0:96], identity=ident), reads=["selb", "cb"], writes=[PSK(7)])
                    S.op("dve", lambda e: e.tensor_copy(out=maddT[pg_:pg_ + 32, g, :, :], in_=mk_ap(psb(7)[pg_:pg_ + 32, 0:1], [[0, 4], [1, 128]])),
                         reads=[PSK(7)], writes=[f"madd{g}"])

                def add_unit(qkf, postf, pvf, hook=None):
                    units.append((qkf, postf, pvf, hook))

                def mk_dil(h, kg, nb, first, last, Ob, slot):
                    hp, pr = h // 2, (h % 2) * 64
                    def qkf(stb):
                        for b in range(nb):
                            S.op("pe", lambda e: e.matmul(ps[:, stb, b * 128:(b + 1) * 128], lhsT=KT[:, hp, (kg + b) * 128:(kg + b + 1) * 128],
                                                          rhs=QZd[:, h, :], start=True, stop=True),
                                 reads=["KT", QK_], writes=[PSK(stb)], inc=(b == nb - 1))
                    def postf(stb, pi, do_exp=True):
                        P = Pb[pi]
                        if do_exp:
                            S.op("act", lambda e: e.activation(out=P[:, 0:nb * 128], in_=ps[:, stb, 0:nb * 128], func=AF.Exp, scale=0.125),
                                 reads=[PSK(stb)], writes=[f"P{pi}"])
                        m0 = 15 - t + kg
                        S.op("dve", lambda e: e.tensor_tensor(out=P[:, 0:nb * 128], in0=P[:, 0:nb * 128],
                                                              in1=cb[:, CB_MALL + m0 * 128:CB_MALL + (m0 + nb) * 128], op=ALU.mult),
                             reads=[f"P{pi}", "cb"], writes=[f"P{pi}"])
                    def pvf(pi):
                        P = Pb[pi]
                        for b in range(nb):
                            S.op("pe", lambda e: e.matmul(ps[:, Ob, slot * 65:(slot + 1) * 65], lhsT=P[:, b * 128:(b + 1) * 128], rhs=Vd[:, kg + b, h, :],
                                                          start=(first and b == 0), stop=(last and b == nb - 1), skip_group_check=True),
                                 reads=[f"P{pi}", "Vd"], writes=[PSK(Ob)], inc=(b == nb - 1))
                    return qkf, postf, pvf

                def dil_fin(quad, Ob):
                    O3 = ps[:, Ob, 0:260].rearrange("p (r e) -> p r e", r=4)
                    S.op("dve", lambda e: e.reciprocal(out=sm[:, 0:4], in_=O3[:, :, 64]), reads=[PSK(Ob)], writes=["sm0"])
                    S.op("dve", lambda e: e.tensor_tensor(out=mixb[:, quad * 256:(quad + 1) * 256].rearrange("p (r e) -> p r e", r=4),
                                                          in0=O3[:, :, 0:64], in1=mk_ap(sm[:, 0:1], [[1, 4], [0, 64]]), op=ALU.mult),
                         reads=[PSK(Ob), "sm0"], writes=[f"ktm{qk}"])

                def mk_nsa(g, kind, j, first, last, Ob):
                    pg = g * 64
                    kp = 127 if kind == "cmp" else 128
                    w = 97 if kind == "cmp" else 65
                    def qkf(stb):
                        if kind == "cmp":
                            lhs = KCT[:, 0:127]
                        else:
                            lhs = KT[:, 4 if kind == "sel" else 5, j * 128:(j + 1) * 128]
                        S.op("pe", lambda e: e.matmul(ps[0:kp, stb, 0:512], lhsT=lhs, rhs=QZn[:, g, :, :].rearrange("p r q -> p (r q)"),
                                                      start=True, stop=(kind != "sel"), skip_group_check=True),
                             reads=["KT", "KCT", QK_], writes=[PSK(stb)], inc=(kind != "sel"))
                        if kind == "sel":
                            S.op("pe", lambda e: e.matmul(ps[:, stb, 0:512], lhsT=cb[:, CB_E + j * 128:CB_E + (j + 1) * 128],
                                                          rhs=maddT[:, g, :, :].rearrange("p r q -> p (r q)"), start=False, stop=True, skip_group_check=True),
                                 reads=["cb", f"madd{g}"], writes=[PSK(stb)])
                    def postf(stb, pi, do_exp=True):
                        P = Pb[pi]
                        if do_exp:
                            S.op("act", lambda e: e.activation(out=P[0:kp, :], in_=ps[0:kp, stb, :], func=AF.Exp, scale=0.125),
                                 reads=[PSK(stb)], writes=[f"P{pi}"])
                        P3 = P[0:kp, :].rearrange("p (r q) -> p r q", r=4)
                        msk = None
                        if kind == "cmp":
                            msk = mk_ap(cb[0:127, CB_CM + t * 128:CB_CM + t * 128 + 1], [[0, 4], [1, 128]])
                        elif j == t:
                            msk = mk_ap(cb[:, CB_TRI:CB_TRI + 1], [[0, 4], [1, 128]])
                        elif kind == "win" and j == t - 4:
                            msk = mk_ap(cb[:, CB_TRIW:CB_TRIW + 1], [[0, 4], [1, 128]])
                        if msk is not None:
                            S.op("dve", lambda e: e.tensor_tensor(out=P3, in0=P3, in1=msk, op=ALU.mult), reads=[f"P{pi}", "cb"], writes=[f"P{pi}"])
                    def pvf(pi):
                        P = Pb[pi]
                        for r in range(4):
                            if kind == "cmp":
                                rhs = VCX[0:127, g, :]
                            else:
                                rhs = VS[:, j, (0 if kind == "sel" else 2) + g, :]
                            S.op("pe", lambda e: e.matmul(ps[:, Ob, r * w:(r + 1) * w], lhsT=P[0:kp, r * 128:(r + 1) * 128], rhs=rhs,
                                                          start=(first and r == 0), stop=(last and r == 3), skip_group_check=True),
                                 reads=[f"P{pi}", "VS", "VCX"], writes=[PSK(Ob)], inc=(r == 3))
                    return qkf, postf, pvf

                for g in range(2):
                    add_unit(*mk_nsa(g, "cmp", 0, True, True, 4 + g),
                             hook=(lambda g=g: (nsa_fin(g, 4 + g, 97, 0, True), select(g), [(4, (lambda g=g: select_b(g)))])[2]))
                for quad in range(2):
                    Ob = 6 if PAIR else 3
                    kgs = list(range(0, t + 1, 4))
                    for hi in range(4):
                        h = quad * 4 + hi
                        for ki, kg in enumerate(kgs):
                            nb = min(4, t + 1 - kg)
                            lastu = (ki == len(kgs) - 1)
                            hk = (lambda quad=quad, Ob=Ob: dil_fin(quad, Ob)) if (hi == 3 and lastu) else None
                            add_unit(*mk_dil(h, kg, nb, ki == 0, lastu, Ob, hi), hook=hk)
                for g in range(2):
                    for j in range(0, t + 1):
                        hk = (lambda g=g: nsa_fin(g, 4 + g, 65, 1, False)) if j == t else None
                        add_unit(*mk_nsa(g, "sel", j, j == 0, j == t, 4 + g), hook=hk)
                for g in range(2):
                    j0 = max(0, t - 4)
                    for j in range(j0, t + 1):
                        def hk_win(g=g):
                            nsa_fin(g, 4 + g, 65, 2, False)
                            S.op("dve", lambda e: e.tensor_copy(out=mixb[:, 512 + g * 256:512 + (g + 1) * 256].rearrange("p (r e) -> p r e", r=4), in_=accg[:, g, :, :]),
                                 reads=[f"accg{g}"], writes=[f"ktm{qk}"])
                        add_unit(*mk_nsa(g, "win", j, j == j0, j == t, 4 + g), hook=(hk_win if j == t else None))
                stages = []
                R = lambda a, b: rs[:, a:b]
                dv = lambda f, rd, wr_: S.op("dve", f, reads=rd, writes=wr_)
                mixb = ktm[qk]
                def stage(f):
                    stages.append(f)
                    return f
                @stage
                def _s0():
                    transposes(mixb, 7, 8, 128, [f"ktm{qk}"])
                    S.op("dve", lambda e: e.tensor_copy(out=mixT[:, :, :], in_=psb(7).rearrange("p (c n) -> p c n", c=8)), reads=[PSK(7)], writes=["mixT"])
                @stage
                def _s1():
                    S.dma("sp", xt[qk][:, :], x_d[s, tsl, :], writes=[f"xt{qk}"])
                    zero_fill_one()
                    for half in range(2):
                        hs_ = slice(half * 512, (half + 1) * 512)
                        for c in range(8):
                            S.op("pe", lambda e: e.matmul(ps[:, 7, :], lhsT=mixT[:, c, :], rhs=wo[:, c, hs_], start=(c == 0), stop=(c == 7)),
                                 reads=["mixT", "wo"], writes=[PSK(7)], inc=(c == 7))
                        S.op("dve", lambda e: e.tensor_tensor(out=xm[:, hs_], in0=ps[:, 7, :], in1=xt[qk][:, hs_], op=ALU.add),
                             reads=[PSK(7), f"xt{qk}"], writes=["xm"])
                @stage
                def _s2():
                    S.dma("sp", xmid_d[s * S_LEN + t * 128:s * S_LEN + (t + 1) * 128, :], xm[:, :], reads=["xm"])
                    S.op("act", lambda e: e.activation(out=h2[:, :], in_=xm[:, :], func=AF.Square, accum_out=st[qk][:, 2:3]),
                         reads=["xm"], writes=["h2", f"ssb{qk}"])
                    rstd(st[qk][:, 2:3], st[qk][:, 3:4], f"ssb{qk}", f"rstdb{qk}")
                    S.op("dve", lambda e: e.scalar_tensor_tensor(out=h2[:, :], in0=xm[:, :], scalar=st[qk][:, 3:4], in1=g2b[:, :], op0=ALU.mult, op1=ALU.mult),
                         reads=["xm", f"rstdb{qk}", "g2b"], writes=["h2"])
                    S.dma("sp", h2s_d[s * S_LEN + t * 128:s * S_LEN + (t + 1) * 128, :], h2[:, :], reads=["h2"])
                @stage
                def _s3():
                    transposes(h2, 7, 8, 128, ["h2"])
                    S.op("dve", lambda e: e.tensor_copy(out=mixT[:, :, :], in_=psb(7).rearrange("p (c n) -> p c n", c=8)), reads=[PSK(7)], writes=["mixT"])
                    for c in range(8):
                        S.op("pe", lambda e: e.matmul(ps[:, 7, 0:72], lhsT=mixT[:, c, :], rhs=wr[:, c, :], start=(c == 0), stop=(c == 7)),
                             reads=["mixT", "wr"], writes=[PSK(7)], inc=(c == 7))
                @stage
                def _s4():
                    dv(lambda e: e.tensor_tensor(out=R(64, 136), in0=ps[:, 7, 0:72], in1=brb[:, :], op=ALU.add), [PSK(7), "brb"], ["lg"])
                    dv(lambda e: e.max(out=R(136, 144), in_=R(64, 72)), ["lg"], ["mg"])
                    dv(lambda e: e.tensor_scalar(out=R(144, 152), in0=R(64, 72), scalar1=R(136, 137), scalar2=None, op0=ALU.is_ge), ["lg", "mg"], ["ohg"])
                    dv(lambda e: e.tensor_scalar_mul(out=R(152, 153), in0=R(136, 137), scalar1=-1.0), ["mg"], ["nmg"])
                    S.op("act", lambda e: e.activation(out=R(160, 168), in_=R(64, 72), func=AF.Exp, bias=R(152, 153), accum_out=R(153, 154)),
                         reads=["lg", "nmg"], writes=["eg", "sg"])
                    dv(lambda e: e.reciprocal(out=R(154, 155), in_=R(153, 154)), ["sg"], ["pg"])
                @stage
                def _s5():
                    el3 = R(72, 136).rearrange("p (g x) -> p g x", g=8)
                    dv(lambda e: e.tensor_tensor(out=R(168, 232).rearrange("p (g x) -> p g x", g=8), in0=el3, in1=mk_ap(R(144, 145), [[1, 8], [0, 8]]), op=ALU.mult),
                       ["lg", "ohg"], ["elm"])
                    dv(lambda e: e.tensor_reduce(out=R(232, 240), in_=R(168, 232).rearrange("p (g x) -> p x g", g=8), axis=AX.X, op=ALU.add), ["elm"], ["els"])
                    dv(lambda e: e.max(out=R(240, 248), in_=R(232, 240)), ["els"], ["m8"])
                    dv(lambda e: e.tensor_tensor(out=R(248, 249), in0=R(241, 242), in1=R(240, 241), op=ALU.subtract), ["m8"], ["dd"])
                    S.op("act", lambda e: e.activation(out=R(249, 250), in_=R(248, 249), func=AF.Exp), reads=["dd"], writes=["r21"])
                    dv(lambda e: e.tensor_scalar_add(out=R(250, 251), in0=R(249, 250), scalar1=1.0), ["r21"], ["den"])
                    dv(lambda e: e.reciprocal(out=R(251, 252), in_=R(250, 251)), ["den"], ["rden"])
                    dv(lambda e: e.tensor_tensor(out=R(252, 253), in0=R(251, 252), in1=R(154, 155), op=ALU.mult), ["rden", "pg"], ["w1v"])
                    dv(lambda e: e.tensor_tensor(out=R(253, 254), in0=R(252, 253), in1=R(249, 250), op=ALU.mult), ["w1v", "r21"], ["w2v"])
                    dv(lambda e: e.tensor_copy(out=WV[:, s, t:t + 1], in_=R(252, 253)), ["w1v"], ["WV"])
                    dv(lambda e: e.tensor_copy(out=WV[:, s, 16 + t:17 + t], in_=R(253, 254)), ["w2v"], ["WV"])
                @stage
                def _s6():
                    for ch in range(2):
                        dv(lambda e: e.tensor_scalar(out=R(40 + ch * 8, 48 + ch * 8), in0=R(232, 240), scalar1=R(240 + ch, 241 + ch), scalar2=None, op0=ALU.is_equal),
                           ["els", "m8", "rs0"], [f"m{ch}"])
                        dv(lambda e: e.tensor_tensor(out=OH[:, ch, t, :].rearrange("p (g x) -> p g x", g=8), in0=mk_ap(R(144, 145), [[1, 8], [0, 8]]),
                                                     in1=mk_ap(R(40 + ch * 8, 41 + ch * 8), [[0, 8], [1, 8]]), op=ALU.mult), ["ohg", f"m{ch}"], [f"OH{t}"])
                    UT = cb[:, CB_UT:CB_UT + 128]
                    ON = cb[:, CB_ONES:CB_ONES + 128]
                    pm = lambda o0, lh, ch, st_, sp_: S.op("pe", lambda e: e.matmul(ps[:, 7, 128 + o0:128 + o0 + 64], lhsT=lh, rhs=OH[:, ch, t, :], start=st_, stop=sp_, skip_group_check=True),
                                                          reads=[f"OH{t}", "cb"], writes=[PSK(7)], inc=sp_)
                    pm(0, UT, 0, True, True)
                    pm(64, UT, 1, True, False)
                    pm(64, ON, 0, False, True)
                    pm(128, ON, 0, True, False)
                    pm(128, ON, 1, False, True)
                @stage
                def _s7():
                    tm2 = tmpb[:, :, :].rearrange("p r e -> p (r e)")[:, 0:128].rearrange("p (c e) -> p c e", c=2)
                    dv(lambda e: e.tensor_tensor(out=tm2, in0=ps[:, 7, 128:256].rearrange("p (c e) -> p c e", c=2), in1=mk_ap(run[:, 0:1], [[0, 2], [1, 64]]), op=ALU.add),
                       [PSK(7), "run"], ["tmpb"])
                    dv(lambda e: e.tensor_tensor(out=tm2, in0=tm2, in1=mk_ap(OH[:, 0, t, 0:1], [[NT * NE, 2], [1, 64]]), op=ALU.mult), ["tmpb", f"OH{t}"], ["tmpb"])
                    dv(lambda e: e.tensor_reduce(out=mk_ap(RK[:, s, t:t + 1], [[16, 2]]), in_=tm2, axis=AX.X, op=ALU.add), ["tmpb"], ["RK"])
                    dv(lambda e: e.tensor_tensor(out=run[:, :], in0=run[:, :], in1=ps[:, 7, 256:320], op=ALU.add), [PSK(7), "run"], ["run"])
                return units, stages

        prev_stages = []
        for t in range(NT):
            NPRE = int(os.environ.get('NPRE', '0'))
            units, stages_t = make_tile(t, prev_stages[0:NPRE])
            prev_stages = prev_stages[NPRE:]
            nst = len(prev_stages)
            gap = max(1, len(units) // (nst + 1)) if nst else 0
            si = 0
            deferred = []
            def after_unit(ui, hook):
                nonlocal_deferred = deferred
                if hook is not None:
                    r_ = hook()
                    if isinstance(r_, list):
                        for (dl_, fn_) in r_:
                            nonlocal_deferred.append((ui + dl_, fn_))
                for d_ in [d_ for d_ in nonlocal_deferred if d_[0] <= ui]:
                    d_[1]()
                    nonlocal_deferred.remove(d_)

            if PAIR:
                nu = len(units)
                bank_of = lambda ui: ((ui // 2) % 2) * 2 + (ui % 2)
                for u0 in range(0, min(2, nu)):
                    units[u0][0](bank_of(u0))
                for p0 in range(0, nu, 2):
                    for u1 in range(p0 + 2, min(p0 + 4, nu)):
                        units[u1][0](bank_of(u1))
                    pr_ = (p0 // 2) % 2
                    npair = min(2, nu - p0)
                    if npair == 2:
                        S.op("act", lambda e: e.activation(out=Pp[pr_][:, :], in_=psflat[:, pr_ * 1024:(pr_ + 1) * 1024], func=AF.Exp, scale=0.125),
                             reads=[PSK(2 * pr_), PSK(2 * pr_ + 1)], writes=[f"P{2 * pr_}", f"P{2 * pr_ + 1}"])
                    for ui in range(p0, p0 + npair):
                        qkf, postf, pvf, hook = units[ui]
                        postf(bank_of(ui), bank_of(ui), do_exp=(npair == 1))
                        pvf(bank_of(ui))
                        after_unit(ui, hook)
                        if nst and (ui + 1) % gap == 0 and si < nst:
                            prev_stages[si]()
                            si += 1
            else:
                LA = 3
                STB = [0, 1, 2, 6]
                for ui, (qkf, postf, pvf, hook) in enumerate(units):
                    if ui == 0:
                        for la in range(min(LA, len(units))):
                            units[la][0](STB[la % 4])
                    if ui + LA < len(units):
                        units[ui + LA][0](STB[(ui + LA) % 4])
                    postf(STB[ui % 4], ui % 4)
                    pvf(ui % 4)
                    after_unit(ui, hook)
                    if nst and (ui + 1) % gap == 0 and si < nst:
                        prev_stages[si]()
                        si += 1
            for (du_, fn_) in deferred:
                fn_()
            while si < nst:
                prev_stages[si]()
                si += 1
            prev_stages = stages_t
        for st_ in prev_stages:
            st_()
        S.barrier()
        S.dma("sp", oh_d[s], OH[:, :, :, :].rearrange("p c t e -> p (c t e)"), reads=[])
        S.barrier()
        if stop_after == "B":
            break
    dv = lambda f, rd, wr_: S.op("dve", f, reads=rd, writes=wr_)
    scrA = R1[:, 0:8192].bitcast(F32)
    scr = scrA[:, 0:1024].rearrange("p (a b) -> p a b", a=16)
    dv(lambda e: e.tensor_tensor(out=scrA.rearrange("p (a b) -> p a b", a=64), in0=mk_ap(run[:, 0:1], [[1, 64], [0, 32]]),
                                 in1=mk_ap(cf[:, CF_THR:CF_THR + 1], [[0, 64], [1, 32]]), op=ALU.is_gt), ["run", "cf"], ["scr"])
    dv(lambda e: e.tensor_reduce(out=ntl[:, :], in_=scrA.rearrange("p (a b) -> p a b", a=64), axis=AX.X, op=ALU.add), ["scr"], ["ntl"])
    dv(lambda e: e.tensor_copy(out=cum[0][:, :], in_=ntl[:, :]), ["ntl"], ["cum0"])
    cur = 0
    for sh in (1, 2, 4, 8, 16, 32):
        a_, b_ = cum[cur], cum[1 - cur]
        dv(lambda e: e.tensor_copy(out=b_[:, 0:sh], in_=a_[:, 0:sh]), [f"cum{cur}"], [f"cum{1 - cur}"])
        dv(lambda e: e.tensor_tensor(out=b_[:, sh:64], in0=a_[:, sh:64], in1=a_[:, 0:64 - sh], op=ALU.add), [f"cum{cur}"], [f"cum{1 - cur}"])
        cur = 1 - cur
    cinc = cum[cur]
    sbase = cum[1 - cur]
    dv(lambda e: e.tensor_tensor(out=sbase[:, :], in0=cinc[:, :], in1=ntl[:, :], op=ALU.subtract), [f"cum{cur}", "ntl"], [f"cum{1 - cur}"])
    dv(lambda e: e.tensor_scalar_mul(out=sbase[:, :], in0=sbase[:, :], scalar1=float(SROWS)), [f"cum{1 - cur}"], [f"cum{1 - cur}"])
    for s in range(nseq):
        S.dma("sp", OH[:, :, :, :].rearrange("p c t e -> p (c t e)"), oh_d[s], writes=["OH"])
        for ch in range(2):
            dv(lambda e: e.tensor_tensor(out=scr, in0=OH[:, ch, :, :], in1=mk_ap(sbase[:, 0:1], [[0, 16], [1, 64]]), op=ALU.mult),
               ["OH", f"cum{1 - cur}"], ["scr"])
            dv(lambda e: e.tensor_reduce(out=EF[:, 0:16], in_=scr, axis=AX.X, op=ALU.add), ["scr"], ["EF"])
            dv(lambda e: e.tensor_tensor(out=EF[:, 16:32], in0=EF[:, 0:16], in1=RK[:, s, ch * 16:(ch + 1) * 16], op=ALU.add), ["EF", "RK"], ["EF"])
            dv(lambda e: e.tensor_copy(out=DESTI[:, s, ch * 16:(ch + 1) * 16], in_=EF[:, 16:32]), ["EF"], ["DESTI"])
    for sc in range(NSLOT // 16):
        dv(lambda e: e.tensor_tensor(out=scr, in0=mk_ap(cinc[:, 0:1], [[0, 16], [1, 64]]),
                                     in1=mk_ap(cf[:, CF_SIDX + sc * 16:CF_SIDX + sc * 16 + 1], [[1, 16], [0, 64]]), op=ALU.is_le),
           [f"cum{cur}", "cf"], ["scr"])
        dv(lambda e: e.tensor_reduce(out=EF[:, sc * 16:(sc + 1) * 16], in_=scr, axis=AX.X, op=ALU.add), ["scr"], ["EF2"])
    dv(lambda e: e.tensor_scalar(out=EF[:, :], in0=EF[:, :], scalar1=128.0, scalar2=cf[:, CF_PIDX:CF_PIDX + 1], op0=ALU.mult, op1=ALU.add),
       ["EF2", "cf", "EF"], ["EF2"])
    dv(lambda e: e.tensor_copy(out=WIDX[:, :], in_=EF[:, :]), ["EF2"], ["WIDX"])
    S.dma("sp", g2b[:, :], gf_d.partition_broadcast(128), writes=["g2b"])
    S.barrier()
    full = (stop_after is None)
    while zf_state["next"] < zf_state["total"]:
        zero_fill_one()
    S.barrier()
    sbuf4 = [d_f32[2 + j].bitcast(BF16).rearrange("p (i n) -> p i n", i=2) for j in range(4)]
    cnt_ = 0
    for s in (range(nseq) if full else []):
        for tp in range(NT // 2):
            j = cnt_ % 4
            cnt_ += 1
            r0 = s * S_LEN + tp * 256
            S.dma("sp", sbuf4[j], h2s_d[r0:r0 + 256, :].rearrange("(i p) d -> p i d", p=128), writes=[f"hst{j}"])
            for ii in range(2):
                t = tp * 2 + ii
                for ch in range(2):
                    S.idma(hs_d[:, :], IndirectOffsetOnAxis(ap=DESTI[:, s, ch * 16 + t:ch * 16 + t + 1], axis=0), sbuf4[j][:, ii, :], None,
                           reads=[f"hst{j}", "DESTI"])
    S.barrier()
    wg_rows, wu_rows, wd_rows = wg_d[:, :], wu_d[:, :], wd_d[:, :]

    def load_weights(slot):
        kw_ = slot % 3
        off = IndirectOffsetOnAxis(ap=WIDX[:, slot:slot + 1], axis=0)
        for (wt, rows, key) in [(wgs[kw_], wg_rows, f"wg{kw_}"), (wus[kw_], wu_rows, f"wu{kw_}"), (wds[kw_], wd_rows, f"wd{kw_}")]:
            S.idma(wt.rearrange("p c n -> p (c n)"), None, rows, off, reads=["WIDX"], writes=[key], bounds_check=bc_reg, oob_is_err=False)

    rowb = [d_bf[0], d_bf[1], R1[:, 65536:67584].bitcast(BF16)]

    def load_rows(hsl):
        k3 = hsl % 3
        S.dma("sp", rowb[k3], hs_d[hsl * 128:(hsl + 1) * 128, :], writes=[f"hsb{k3}"])

    nh = NHALF if full else 0
    if full:
        load_weights(0)
        load_weights(1)
        load_rows(0)
        load_rows(1)
    for hsl in range(nh):
        k = hsl % 2
        slot = hsl // 2
        kw_ = slot % 3
        if hsl + 1 < nh:
            if hsl % 2 == 0 and slot + 2 < NSLOT:
                load_weights(slot + 2)
            if hsl + 2 < nh:
                load_rows(hsl + 2)
        hT = d_bf[2 + k].rearrange("p (c n) -> p c n", c=8)
        transposes(rowb[hsl % 3], 6 + k, 8, 128, [f"hsb{hsl % 3}"])
        S.op("dve", lambda e: e.tensor_copy(out=hT, in_=psb(6 + k).rearrange("p (c n) -> p c n", c=8)), reads=[PSK(6 + k)], writes=[f"hT{k}"])
        gub = k
        for gi, (wt, wk) in enumerate([(wgs[kw_], f"wg{kw_}"), (wus[kw_], f"wu{kw_}")]):
            for f in range(2):
                col = gi * 256 + f * 128
                for c in range(8):
                    S.op("pe", lambda e: e.matmul(ps[:, gub, col:col + 128], lhsT=wt[:, c, f * 128:(f + 1) * 128], rhs=hT[:, c, :], start=(c == 0), stop=(c == 7)),
                         reads=[wk, f"hT{k}"], writes=[PSK(gub)], inc=(c == 7))
        sgs = d_f32[8 + k][:, 0:256]
        heT = d_bf[4 + k][:, 0:256]
        S.op("act", lambda e: e.activation(out=sgs, in_=ps[:, gub, 0:256], func=AF.Silu), reads=[PSK(gub)], writes=[f"sgs{k}"])
        S.op("dve", lambda e: e.tensor_tensor(out=heT, in0=sgs, in1=ps[:, gub, 256:512], op=ALU.mult), reads=[f"sgs{k}", PSK(gub)], writes=[f"heT{k}"])
        yb = 2 + 2 * k
        for half in range(2):
            for f in range(2):
                S.op("pe", lambda e: e.matmul(ps[:, yb + half, :], lhsT=heT[:, f * 128:(f + 1) * 128], rhs=wds[kw_][:, f, half * 512:(half + 1) * 512],
                                              start=(f == 0), stop=(f == 1)),
                     reads=[f"heT{k}", f"wd{kw_}"], writes=[PSK(yb + half)], inc=(f == 1))
        ysb = d_f32[k]
        S.op("act", lambda e: e.copy(out=ysb[:, 0:512], in_=ps[:, yb, :]), reads=[PSK(yb)], writes=[f"ysb{k}"])
        S.op("dve", lambda e: e.tensor_copy(out=ysb[:, 512:1024], in_=ps[:, yb + 1, :]), reads=[PSK(yb + 1)], writes=[f"ysb{k}"])
        S.dma("act", ys_d[hsl * 128:(hsl + 1) * 128, :], ysb, reads=[f"ysb{k}"])
    S.barrier()
    ntile_all = nseq * NT if full else 0

    def cmb_load(idx):
        s_, i_ = divmod(idx, NT)
        j = idx % 3
        S.dma("sp", d_f32[6 + j], xmid_d[s_ * S_LEN + i_ * 128:s_ * S_LEN + (i_ + 1) * 128, :], writes=[f"xmt{j}"])
        S.idma(d_f32[j], None, ys_d[:, :], IndirectOffsetOnAxis(ap=DESTI[:, s_, i_:i_ + 1], axis=0), reads=["DESTI"], writes=[f"y1{j}"])
        S.idma(d_f32[3 + j], None, ys_d[:, :], IndirectOffsetOnAxis(ap=DESTI[:, s_, 16 + i_:17 + i_], axis=0), reads=["DESTI"], writes=[f"y2{j}"])

    for idx in range(min(2, ntile_all)):
        cmb_load(idx)
    for idx in range(ntile_all):
        s_, i = divmod(idx, NT)
        j = idx % 3
        k = idx % 2
        if idx + 2 < ntile_all:
            cmb_load(idx + 2)
        y1, y2, xmt = d_f32[j], d_f32[3 + j], d_f32[6 + j]
        S.op("dve", lambda e: e.scalar_tensor_tensor(out=xmt, in0=y1, scalar=WV[:, s_, i:i + 1], in1=xmt, op0=ALU.mult, op1=ALU.add),
             reads=[f"y1{j}", f"xmt{j}", "WV"], writes=[f"xmt{j}"])
        S.op("dve", lambda e: e.scalar_tensor_tensor(out=xmt, in0=y2, scalar=WV[:, s_, 16 + i:17 + i], in1=xmt, op0=ALU.mult, op1=ALU.add),
             reads=[f"y2{j}", f"xmt{j}", "WV"], writes=[f"xmt{j}"])
        S.op("act", lambda e: e.activation(out=d_bf[k], in_=xmt, func=AF.Square, accum_out=st[k][:, 4:5]),
             reads=[f"xmt{j}"], writes=[f"dbf{k}", f"ssf{k}"])
        rstd(st[k][:, 4:5], st[k][:, 5:6], f"ssf{k}", f"rstdf{k}")
        S.op("dve", lambda e: e.scalar_tensor_tensor(out=y1, in0=xmt, scalar=st[k][:, 5:6], in1=g2b[:, :], op0=ALU.mult, op1=ALU.mult),
             reads=[f"xmt{j}", f"rstdf{k}", "g2b"], writes=[f"y1{j}"])
        S.dma("act", out_d[s_, i * 128:(i + 1) * 128, :], y1, reads=[f"y1{j}"])
    S.barrier()
    if debug:
        for name, (ap, shape, dty) in debug_taps(locals()).items():
            d = dbg_out(name, shape, dty)
            S.dma("sp", d, ap, reads=[])
        S.barrier()
    print(f"[build] instructions={S.n_ins} waits={S.n_wait} counts={S.cnt} sbuf_free={nc.sbuf_bytes_remaining}")
    return nc, dbg


def debug_taps(L):
    return {
        "KT": (L["KT"][:, :, :], [128, 8, S_LEN], BF16),
        "Vd": (L["Vd"][:, :, :, :], [128, NT, 8, 65], BF16),
        "VS": (L["VS"][:, :, :, :], [128, NT, 4, 65], BF16),
        "KCT": (L["KCT"][:, :], [128, 128], BF16),
        "VCX": (L["VCX"][:, :, :], [128, 2, 97], BF16),
        "xT": (L["xT"][:, :, :], [128, 8, S_LEN], BF16),
        "mix": (L["mix"][:, :], [128, D], BF16),
        "OH": (L["OH"][:, :, :, :], [128, 2, NT, NE], BF16),
        "WV": (L["WV"][:, :, :], [128, 4, 32], F32),
        "DESTI": (L["DESTI"][:, :, :], [128, 4, 32], I32),
        "WIDX": (L["WIDX"][:, :], [128, NSLOT], I32),
        "gate": (L["gate"][:, :], [128, 24], F32),
    }


_CACHE = {}


def prep_weights(norm1_g, w_in, pe_kc, w_kc1, w_kc2, pe_vc, w_vc1, w_vc2, w_o, norm2_g, w_rg, b_rg, w_re, b_re,
                 w_gate, w_up, w_down, norm_f_g):
    f = lambda a: np.ascontiguousarray(np.asarray(a, dtype=np.float32))
    kvp, qp = w_in_perm()
    w_in0 = f(w_in)[0]
    pm = lambda w: np.ascontiguousarray(w.reshape(8, 128, -1).transpose(1, 0, 2))
    cbv, cfv = host_constants()
    peT = np.zeros((128, 2, 32), np.float32)
    for kv, pe in enumerate((f(pe_kc)[0], f(pe_vc)[0])):
        peT[0:64, kv, :] = pe.T
        peT[64:128, kv, :] = pe.T
    w1 = np.stack([f(w_kc1)[0], f(w_vc1)[0]])
    w1 = w1.reshape(2, 32, 64, 256).transpose(0, 2, 1, 3)
    w1 = np.ascontiguousarray(np.concatenate([w1, w1], axis=1))
    w2 = np.stack([f(w_kc2)[0], f(w_vc2)[0]])
    w2 = np.ascontiguousarray(w2.reshape(2, 2, 128, 64).transpose(0, 2, 1, 3))
    w_r = np.concatenate([f(w_rg)[0], f(w_re)[0].reshape(D, 64)], axis=1)
    b_r = np.concatenate([f(b_rg)[0], f(b_re)[0].reshape(64)])
    wg = np.ascontiguousarray(f(w_gate)[0].reshape(NE, 8, 128, DFF).transpose(0, 2, 1, 3)).reshape(NE * 128, 8 * DFF)
    wu = np.ascontiguousarray(f(w_up)[0].reshape(NE, 8, 128, DFF).transpose(0, 2, 1, 3)).reshape(NE * 128, 8 * DFF)
    wd = np.ascontiguousarray(f(w_down)[0].reshape(NE, 2, 128, D).transpose(0, 2, 1, 3)).reshape(NE * 128, 2 * D)
    return {
        "w_kv": pm(w_in0[:, kvp]), "w_q": pm(w_in0[:, qp]), "w_o": pm(f(w_o)[0]), "w_r": pm(w_r), "b_r": b_r,
        "g1": f(norm1_g)[0], "g2": f(norm2_g)[0], "gf": f(norm_f_g), "peT": peT, "w_c1": w1, "w_c2": w2,
        "w_gate": wg, "w_up": wu, "w_down": wd, "cb": cbv, "cf": cfv,
    }


def kernel(x, norm1_g, w_in, pe_kc, w_kc1, w_kc2, pe_vc, w_vc1, w_vc2, w_o, norm2_g, w_rg, b_rg, w_re, b_re,
           w_gate, w_up, w_down, norm_f_g):
    x = np.asarray(x, dtype=np.float32)
    nseq = B_TOTAL // N_CORES
    W = prep_weights(norm1_g, w_in, pe_kc, w_kc1, w_kc2, pe_vc, w_vc1, w_vc2, w_o, norm2_g, w_rg, b_rg, w_re, b_re,
                     w_gate, w_up, w_down, norm_f_g)
    nc, _ = build(nseq)
    in_maps = []
    for c in range(N_CORES):
        m = dict(W)
        m["x"] = np.ascontiguousarray(x[c * nseq:(c + 1) * nseq])
        in_maps.append(m)
    res = run_bass_kernel_spmd(nc, in_maps, core_ids=list(range(N_CORES)))
    return np.concatenate([np.asarray(r["out"], dtype=np.float32) for r in res.results], axis=0)
```
